# Optimizing a Trainium2 kernel written in Bass

```python
import math
import jax, jax.numpy as jnp
from jax import lax
import numpy as np

D_MODEL = 1024
BATCH = 2
SEQ = 16384
DEPTH = 1

EPS = 1e-6
CONV_WIDTH = D_MODEL
CONV_TAPS = 31
DIFF_HEADS = 8
DIFF_HEAD_DIM = 64
DIFF_V_DIM = 2 * DIFF_HEAD_DIM
QK_WIDTH = DIFF_HEADS * 2 * DIFF_HEAD_DIM
ATTN_WIDTH = DIFF_HEADS * DIFF_V_DIM
ROPE_THETA = 500000.0
ROPE_DIM = DIFF_HEAD_DIM // 4
Q_BLOCK = 128
IN_SIZES = (CONV_WIDTH, CONV_WIDTH, QK_WIDTH, QK_WIDTH, ATTN_WIDTH, D_MODEL, D_MODEL)
IN_WIDTH = 2 * CONV_WIDTH + 2 * QK_WIDTH + ATTN_WIDTH + 2 * D_MODEL
N_EXPERTS = 64
N_GROUPS = 8
TOPK_GROUPS = 4
TOP_K = 8
EXPERT_DIM = 256
SHARED_DIM = 256
ROUTED_SCALE = 2.5
MOE_CHUNK = 128

kernel_name = 'hybrid_conv_diffattn_moe_block'


def rms_norm(x, g):
    x32 = x.astype(jnp.float32)
    y = x32 * lax.rsqrt(jnp.mean(x32 * x32, axis=-1, keepdims=True) + EPS)
    return y.astype(x.dtype) * g


def layer_norm(x, g, b):
    x32 = x.astype(jnp.float32)
    mu = jnp.mean(x32, axis=-1, keepdims=True)
    xc = x32 - mu
    y = xc * lax.rsqrt(jnp.mean(xc * xc, axis=-1, keepdims=True) + EPS)
    return y.astype(x.dtype) * g + b


def rope(x, cos, sin):
    half = ROPE_DIM // 2
    x1, x2, xp = x[..., :half], x[..., half:ROPE_DIM], x[..., ROPE_DIM:]
    return jnp.concatenate([x1 * cos - x2 * sin, x2 * cos + x1 * sin, xp], axis=-1)


def conformer_conv(a, b, w_dw, b_dw, ln_g, ln_b, w_pw2, b_pw2):
    u = a * jax.nn.sigmoid(b)
    u = lax.conv_general_dilated(
        u, w_dw[:, None, :], window_strides=(1,), padding=[(CONV_TAPS - 1, 0)],
        dimension_numbers=('NWC', 'WIO', 'NWC'), feature_group_count=CONV_WIDTH) + b_dw
    u = jax.nn.silu(layer_norm(u, ln_g, ln_b))
    return u @ w_pw2 + b_pw2


def diff_attention(q, k, v, cos, sin, q_g, k_g, lq1, lk1, lq2, lk2, subln_g, lambda_init):
    B, S, _ = q.shape
    H, d = DIFF_HEADS, DIFF_HEAD_DIM
    q = q.reshape(B, S, H, 2, d).transpose(0, 2, 3, 1, 4)
    k = k.reshape(B, S, H, 2, d).transpose(0, 2, 3, 1, 4)
    v = v.reshape(B, S, H, DIFF_V_DIM).transpose(0, 2, 1, 3)
    q = rope(rms_norm(q, q_g), cos, sin)
    k = rope(rms_norm(k, k_g), cos, sin)
    f32 = jnp.float32
    lam = (jnp.exp(jnp.sum(lq1.astype(f32) * lk1.astype(f32)))
           - jnp.exp(jnp.sum(lq2.astype(f32) * lk2.astype(f32))) + lambda_init)
    nb = S // Q_BLOCK
    q_blocks = jnp.moveaxis(q.reshape(B, H, 2, nb, Q_BLOCK, d), 3, 0)
    starts = jnp.arange(nb, dtype=jnp.int32) * Q_BLOCK
    k_idx = jnp.arange(S, dtype=jnp.int32)
    scale = d ** -0.5

    def block(args):
        qb, start = args
        s = jnp.einsum('bhmqd,bhmkd->bhmqk', qb, k).astype(f32) * scale
        causal = k_idx[None, :] <= (start + jnp.arange(Q_BLOCK, dtype=jnp.int32))[:, None]
        p = jax.nn.softmax(jnp.where(causal, s, -jnp.inf), axis=-1)
        a = p[:, :, 0] - lam * p[:, :, 1]
        return jnp.einsum('bhqk,bhkv->bhqv', a.astype(v.dtype), v)

    o = lax.map(block, (q_blocks, starts))
    o = jnp.moveaxis(o, 0, 2).reshape(B, H, S, DIFF_V_DIM)
    o = rms_norm(o, subln_g) * (1.0 - lambda_init)
    return o.transpose(0, 2, 1, 3).reshape(B, S, ATTN_WIDTH)


def moe_ffn(h, w_router, router_bias, w_gu, w_down, w_sh_gu, w_sh_down):
    B, S, D = h.shape
    f32 = jnp.float32
    t = h.reshape(B * S, D)
    scores = jax.nn.sigmoid((t @ w_router).astype(f32))
    choice = scores + router_bias.astype(f32)
    per_group = N_EXPERTS // N_GROUPS
    group_score = lax.top_k(choice.reshape(-1, N_GROUPS, per_group), 2)[0].sum(-1)
    _, gidx = lax.top_k(group_score, TOPK_GROUPS)
    gmask = jax.nn.one_hot(gidx, N_GROUPS, dtype=f32).sum(-2)
    emask = jnp.repeat(gmask, per_group, axis=-1) > 0
    _, eidx = lax.top_k(jnp.where(emask, choice, -jnp.inf), TOP_K)
    w = jnp.take_along_axis(scores, eidx, axis=-1)
    w = w / jnp.sum(w, axis=-1, keepdims=True) * ROUTED_SCALE
    gates = jnp.einsum('tk,tke->te', w, jax.nn.one_hot(eidx, N_EXPERTS, dtype=f32)).astype(h.dtype)
    nc = t.shape[0] // MOE_CHUNK

    def chunk(args):
        tc, gc = args
        g, u = jnp.split(jnp.einsum('td,edf->tef', tc, w_gu), 2, axis=-1)
        return jnp.einsum('tef,efd->td', jax.nn.silu(g) * u * gc[..., None], w_down)

    routed = lax.map(chunk, (t.reshape(nc, MOE_CHUNK, D),
                             gates.reshape(nc, MOE_CHUNK, N_EXPERTS))).reshape(B * S, D)
    sg, su = jnp.split(t @ w_sh_gu, 2, axis=-1)
    shared = (jax.nn.silu(sg) * su) @ w_sh_down
    return (routed + shared).reshape(B, S, D)


def setup_inputs(seed: int = 0) -> dict:
    key = jax.random.key(seed)
    ks = jax.random.split(key, 32)
    L, D, C = DEPTH, D_MODEL, CONV_WIDTH
    f32 = jnp.float32

    def nrm(k, shape, scale):
        return jax.random.normal(k, shape, f32) * scale

    def gain(k, shape):
        return 1.0 + 0.05 * jax.random.normal(k, shape, f32)

    return {
        'x': nrm(ks[0], (BATCH, SEQ, D), 1.0),
        'c': nrm(ks[1], (BATCH, D), 1.0),
        'positions': jnp.broadcast_to(jnp.arange(SEQ, dtype=jnp.int32), (BATCH, SEQ)),
        'w_ada': nrm(ks[2], (L, D, 6 * D), 0.5 * D ** -0.5),
        'b_ada': nrm(ks[3], (L, 6 * D), 0.02),
        'norm1_g': gain(ks[4], (L, D)),
        'w_in': nrm(ks[5], (L, D, IN_WIDTH), D ** -0.5),
        'conv_dw': nrm(ks[6], (L, CONV_TAPS, C), CONV_TAPS ** -0.5),
        'conv_dw_b': nrm(ks[7], (L, C), 0.02),
        'conv_ln_g': gain(ks[8], (L, C)),
        'conv_ln_b': nrm(ks[9], (L, C), 0.02),
        'w_pw2': nrm(ks[10], (L, C, D), C ** -0.5),
        'b_pw2': nrm(ks[11], (L, D), 0.02),
        'q_norm_g': gain(ks[12], (L, DIFF_HEAD_DIM)),
        'k_norm_g': gain(ks[13], (L, DIFF_HEAD_DIM)),
        'lambda_q1': nrm(ks[14], (L, DIFF_HEAD_DIM), 0.1),
        'lambda_k1': nrm(ks[15], (L, DIFF_HEAD_DIM), 0.1),
        'lambda_q2': nrm(ks[16], (L, DIFF_HEAD_DIM), 0.1),
        'lambda_k2': nrm(ks[17], (L, DIFF_HEAD_DIM), 0.1),
        'subln_g': gain(ks[18], (L, DIFF_V_DIM)),
        'w_out': nrm(ks[19], (L, D, D), D ** -0.5),
        'norm2_g': gain(ks[20], (L, D)),
        'w_router': nrm(ks[21], (L, D, N_EXPERTS), D ** -0.5),
        'router_bias': nrm(ks[22], (L, N_EXPERTS), 0.01),
        'w_exp_gu': nrm(ks[23], (L, N_EXPERTS, D, 2 * EXPERT_DIM), D ** -0.5),
        'w_exp_down': nrm(ks[24], (L, N_EXPERTS, EXPERT_DIM, D), EXPERT_DIM ** -0.5),
        'w_sh_gu': nrm(ks[25], (L, D, 2 * SHARED_DIM), D ** -0.5),
        'w_sh_down': nrm(ks[26], (L, SHARED_DIM, D), SHARED_DIM ** -0.5),
    }


def reference(x, c, positions, w_ada, b_ada, norm1_g, w_in, conv_dw, conv_dw_b, conv_ln_g,
              conv_ln_b, w_pw2, b_pw2, q_norm_g, k_norm_g, lambda_q1, lambda_k1, lambda_q2,
              lambda_k2, subln_g, w_out, norm2_g, w_router, router_bias, w_exp_gu, w_exp_down,
              w_sh_gu, w_sh_down):
    inv_freq = ROPE_THETA ** (-jnp.arange(0, ROPE_DIM, 2, dtype=jnp.float32) / ROPE_DIM)
    ang = positions.astype(jnp.float32)[:, None, None, :, None] * inv_freq
    cos = jnp.cos(ang).astype(x.dtype)
    sin = jnp.sin(ang).astype(x.dtype)
    c_act = jax.nn.silu(c)
    offsets = []
    acc = 0
    for s in IN_SIZES[:-1]:
        acc += s
        offsets.append(acc)
    for l in range(DEPTH):
        lambda_init = 0.8 - 0.6 * math.exp(-0.3 * l)
        mod = c_act @ w_ada[l] + b_ada[l]
        sh1, sc1, g1, sh2, sc2, g2 = [m[:, None, :] for m in jnp.split(mod, 6, axis=-1)]
        h = rms_norm(x, norm1_g[l]) * (1 + sc1) + sh1
        proj = h @ w_in[l]
        ca, cb, q, k, v, gate_conv, gate_attn = jnp.split(proj, offsets, axis=-1)
        y_conv = conformer_conv(ca, cb, conv_dw[l], conv_dw_b[l], conv_ln_g[l], conv_ln_b[l],
                                w_pw2[l], b_pw2[l])
        y_attn = diff_attention(q, k, v, cos, sin, q_norm_g[l], k_norm_g[l], lambda_q1[l],
                                lambda_k1[l], lambda_q2[l], lambda_k2[l], subln_g[l], lambda_init)
        merged = jax.nn.sigmoid(gate_conv) * y_conv + jax.nn.sigmoid(gate_attn) * y_attn
        x = x + g1 * (merged @ w_out[l])
        h2 = rms_norm(x, norm2_g[l]) * (1 + sc2) + sh2
        x = x + g2 * moe_ffn(h2, w_router[l], router_bias[l], w_exp_gu[l], w_exp_down[l],
                             w_sh_gu[l], w_sh_down[l])
    return x
```

```python
import math
from contextlib import ExitStack
import numpy as np
import ml_dtypes
import concourse.bass as bass
import concourse.mybir as mybir
from concourse.bass_utils import run_bass_kernel_spmd

F32 = mybir.dt.float32
BF16 = mybir.dt.bfloat16
I32 = mybir.dt.int32
AF = mybir.ActivationFunctionType
ALU = mybir.AluOpType
AX = mybir.AxisListType

D = 1024
S = 16384
EPS = 1e-6
NEXP = 65
TWO_PI = 2.0 * math.pi

C_C, C_BADA, C_N1G, C_N2G, C_DWB, C_LNG, C_LNB, C_BPW2, C_DW, C_HALO, C_INVF, C_SUBLN = 0, 8, 56, 64, 72, 80, 88, 96, 104, 352, 353, 361
NCOL = 362
R_QG, R_KG, R_SUBLN, R_LAM, R_RB = 0, 64, 128, 256, 512
NROW = 576


class Buf:
    __slots__ = ("w", "r", "name")

    def __init__(self, name=""):
        self.w = None
        self.r = {}
        self.name = name


class Builder:
    def __init__(self, cfg):
        self.cfg = cfg
        self.nc = bass.Bass("TRN2", target_bir_lowering=False)
        nc = self.nc
        self.root = ExitStack()
        self.eng = {"pe": nc.tensor, "act": nc.scalar, "dve": nc.vector, "pool": nc.gpsimd, "sp": nc.sync}
        self.prog = {e: self.root.enter_context(nc.semaphore("prog_" + e)) for e in ("pe", "act", "dve", "pool")}
        self.cnt = {e: 0 for e in self.prog}
        self.waited = {}
        self.dsems = []
        self.free_dsems = []
        self.uid = 0

    def sig(self, e, ins):
        self.cnt[e] += 1
        ins.then_inc(self.prog[e], 1)
        return ("c", e, self.cnt[e])

    def wait(self, e, tok, force=False):
        if tok is None:
            return
        if tok[0] == "c":
            _, p, c = tok
            if p == e and e == "pe" and not force:
                return
            key = (e, "c", p)
            if self.waited.get(key, 0) >= c:
                return
            self.eng[e].wait_ge(self.prog[p], c)
            self.waited[key] = c
        else:
            _, sid, c = tok
            key = (e, "d", sid)
            if self.waited.get(key, 0) >= c:
                return
            self.eng[e].wait_ge(self.dsems[sid][0], c)
            self.waited[key] = c

    def _pre(self, e, R, W, SR):
        R, W, SR = [getattr(b, "b", b) for b in R], [getattr(b, "b", b) for b in W], [getattr(b, "b", b) for b in SR]
        for b in R:
            self.wait(e, b.w)
        for b in SR:
            self.wait(e, b.w, force=True)
        for b in W:
            self.wait(e, b.w)
            for t in b.r.values():
                self.wait(e, t)

    def _post(self, tok, key, R, W, SR):
        R, W, SR = [getattr(b, "b", b) for b in R], [getattr(b, "b", b) for b in W], [getattr(b, "b", b) for b in SR]
        for b in R:
            b.r[key] = tok
        for b in SR:
            b.r[key] = tok
        for b in W:
            b.w = tok
            b.r = {}

    def op(self, e, fn, R=(), W=(), SR=()):
        self._pre(e, R, W, SR)
        ins = fn()
        tok = self.sig(e, ins)
        self._post(tok, ("c", e), R, W, SR)
        return tok

    def new_dsem(self):
        sem = self.root.enter_context(self.nc.semaphore("dsem%d" % len(self.dsems)))
        self.dsems.append([sem, 0])
        return len(self.dsems) - 1

    def dma(self, q, sid, out, in_, R=(), W=()):
        self._pre(q, R, W, ())
        ins = self.eng[q].dma_start(out=out, in_=in_)
        self.dsems[sid][1] += 16
        ins.then_inc(self.dsems[sid][0], 16)
        tok = ("d", sid, self.dsems[sid][1])
        self._post(tok, ("d", sid), R, W, ())
        return tok

    def barrier(self):
        toks = [("c", e, self.cnt[e]) for e in self.prog if self.cnt[e] > 0]
        toks += [("d", i, d[1]) for i, d in enumerate(self.dsems) if d[1] > 0 and i not in getattr(self, "nobar", ())]
        for e in self.eng:
            for t in toks:
                self.wait(e, t)

    def sb(self, st, name, shape, dt):
        self.uid += 1
        return st.enter_context(self.nc.sbuf_tensor("%s_%d" % (name, self.uid), list(shape), dt))

    def ps(self, st, name, shape, dt):
        self.uid += 1
        return st.enter_context(self.nc.psum_tensor("%s_%d" % (name, self.uid), list(shape), dt))

    def declare(self):
        nc, cfg = self.nc, self.cfg

        def din(name, shape, dt=F32):
            return nc.dram_tensor(name, list(shape), dt, kind="ExternalInput").ap()

        def dscr(name, shape, dt):
            return nc.dram_tensor(name, list(shape), dt).ap()

        self.xfull = din("xfull", [S, D])
        self.xown = din("xown", [8, 542, D])
        self.colv_d = din("colv", [128, NCOL])
        self.rowv_d = din("rowv", [128, NROW])
        self.posf_d = din("posf", [128, 128], I32)
        self.poso_d = din("poso", [128, 32], I32)
        self.cmask_d = din("cmask", [128, 16, 512], BF16)
        self.idf_d = din("idf", [128, 128])
        self.w_ada = din("w_ada", [D, 6 * D])
        self.w_in = din("w_in", [D, 7 * D])
        self.w_pw2 = din("w_pw2", [D, D])
        self.w_out = din("w_out", [D, D])
        self.w_router = din("w_router", [D, 64])
        self.w_gu = din("w_gu", [NEXP, D, 512])
        self.w_dn = din("w_dn", [NEXP, 256, D])
        self.y = nc.dram_tensor("y", [4096, D], F32, kind="ExternalOutput").ap()
        self.winb = dscr("winb", [D, 7 * D], BF16)
        self.wpw2b = dscr("wpw2b", [D, D], BF16)
        self.woutb = dscr("woutb", [D, D], BF16)
        self.wgub = dscr("wgub", [NEXP, D, 512], BF16)
        self.wdnb = dscr("wdnb", [NEXP, 256, D], BF16)
        self.KT = dscr("KT", [8, 128, S], BF16)
        self.VS = dscr("VS", [8, 128, 128, 128], BF16)
        self.x1s = dscr("x1s", [4096, D], F32)
        self.h2Ts = dscr("h2Ts", [128, 8, 4096], BF16)
        self.gTs = dscr("gTs", [NEXP, 4096], F32)
        self.dbg = {}
        for name, shape, dt in cfg.get("debug", []):
            self.dbg[name] = nc.dram_tensor("dbg_" + name, list(shape), dt, kind="ExternalOutput").ap()

    def phase0(self):
        nc, st = self.nc, self.root
        V, A, P, T = nc.vector, nc.scalar, nc.gpsimd, nc.tensor
        op, dma = self.op, self.dma
        sA, sE = self.new_dsem(), self.new_dsem()
        bA, bE = Buf("convA"), Buf("convE")
        for r in range(8):
            dma("pool", sA, self.winb[r * 128:(r + 1) * 128, :], self.w_in[r * 128:(r + 1) * 128, :], W=[bA])
        for r in range(2):
            dma("pool", sA, self.wpw2b[r * 512:(r + 1) * 512, :], self.w_pw2[r * 512:(r + 1) * 512, :], W=[bA])
            dma("pool", sA, self.woutb[r * 512:(r + 1) * 512, :], self.w_out[r * 512:(r + 1) * 512, :], W=[bA])
        self.bA, self.bE, self.sE = bA, bE, sE
        self.nobar = {sE}
        self.colv = self.sb(st, "colv", [128, NCOL], F32)
        self.rowv = self.sb(st, "rowv", [128, NROW], F32)
        self.idf = self.sb(st, "idf", [128, 128], F32)
        self.idb = self.sb(st, "idb", [128, 128], BF16)
        self.onesf = self.sb(st, "onesf", [128, 128], F32)
        self.onesb = self.sb(st, "onesb", [128, 128], BF16)
        self.cmask = self.sb(st, "cmask", [128, 16, 512], BF16)
        self.posf_i = self.sb(st, "posf_i", [128, 128], I32)
        self.poso_i = self.sb(st, "poso_i", [128, 32], I32)
        self.epsc = self.sb(st, "epsc", [128, 1], F32)
        self.modc = self.sb(st, "modc", [128, 48], F32)
        self.gmod = self.sb(st, "gmod", [128, 16], F32)
        self.g1bc = self.sb(st, "g1bc", [128, D], F32)
        self.g2bc = self.sb(st, "g2bc", [128, D], F32)
        self.wr = self.sb(st, "wr", [128, 8, 64], F32)
        self.wrh = self.sb(st, "wrh", [128, 8, 64], BF16)
        self.wrl = self.sb(st, "wrl", [128, 8, 64], BF16)
        self.rbias = self.sb(st, "rbias", [128, 64], F32)
        self.lam = self.sb(st, "lam", [128, 4], F32)
        self.sino = self.sb(st, "sino", [128, 32, 8], F32)
        self.coso = self.sb(st, "coso", [128, 32, 8], F32)
        self.p01 = ExitStack()
        self.sinf = self.sb(self.p01, "sinf", [128, 128, 8], F32)
        self.cosf = self.sb(self.p01, "cosf", [128, 128, 8], F32)
        sc = self.new_dsem()
        bC = Buf("consts")
        self.bC = bC
        dma("sp", sc, self.colv[:], self.colv_d, W=[bC])
        dma("sp", sc, self.rowv[:], self.rowv_d, W=[bC])
        dma("sp", sc, self.idf[:], self.idf_d, W=[bC])
        dma("sp", sc, self.cmask[:], self.cmask_d, W=[bC])
        dma("sp", sc, self.posf_i[:], self.posf_d, W=[bC])
        dma("sp", sc, self.wr[:], self.w_router.rearrange("(c p) e -> p c e", p=128), W=[bC])
        dma("sp", sc, self.poso_i[:], self.poso_d, W=[bC])
        colv, rowv = self.colv, self.rowv
        with ExitStack() as ph:
            cact = self.sb(ph, "cact", [128, 8], F32)
            wada = [self.sb(ph, "wada%d" % k, [128, 8, D], F32) for k in range(2)]
            modps = self.ps(ph, "modps", [128, 512], F32)
            bcps = self.ps(ph, "bcps", [128, 2, 512], F32)
            diag = self.sb(ph, "diag", [128, 8, 128], F32)
            wtmp = self.sb(ph, "wtmp", [128, 8, 64], F32)
            tmpf = self.sb(ph, "tmpf", [128, 128, 8], F32)
            tmpi = self.sb(ph, "tmpi", [128, 128, 8], I32)
            tmpk = self.sb(ph, "tmpk", [128, 128, 8], F32)
            tmpf2 = self.sb(ph, "tmpf2", [128, 128, 8], F32)
            posf = self.sb(ph, "posf", [128, 128], F32)
            lt = self.sb(ph, "lt", [128, 128], F32)
            bcact, bmodps, bmodc, bdiag, bbc = Buf(), Buf(), Buf(), Buf(), Buf()
            bwada = [Buf(), Buf()]
            swada = [self.new_dsem(), self.new_dsem()]
            op("dve", lambda: V.memset(self.epsc[:], EPS))
            op("dve", lambda: V.memset(self.onesf[:], 1.0))
            op("dve", lambda: V.memset(self.onesb[:], 1.0))
            op("dve", lambda: V.tensor_copy(out=self.idb[:], in_=self.idf[:]), R=[bC])
            op("act", lambda: A.activation(out=cact[:], in_=colv[:, C_C:C_C + 8], func=AF.Silu), R=[bC], W=[bcact])
            for m in range(6):
                dma("sp", swada[m % 2], wada[m % 2][:],
                    self.w_ada[:, m * D:(m + 1) * D].rearrange("(k p) n -> p k n", p=128), W=[bwada[m % 2]])

                def mm(m=m):
                    last = None
                    for c in range(8):
                        for k in range(8):
                            last = T.matmul(modps[:, m * 8 + c:m * 8 + c + 1], wada[m % 2][:, k, c * 128:(c + 1) * 128],
                                            cact[:, k:k + 1], start=(k == 0), stop=(k == 7))
                    return last
                op("pe", mm, R=[bwada[m % 2], bcact], W=[bmodps])
            op("dve", lambda: V.tensor_tensor(out=self.modc[:], in0=modps[:, 0:48], in1=colv[:, C_BADA:C_BADA + 48], op=ALU.add),
               R=[bmodps, bC], W=[bmodc])
            op("dve", lambda: V.scalar_tensor_tensor(out=self.gmod[:, 0:8], in0=self.modc[:, 8:16], scalar=1.0, in1=colv[:, C_N1G:C_N1G + 8],
                                                     op0=ALU.add, op1=ALU.mult), W=[bmodc])
            op("dve", lambda: V.scalar_tensor_tensor(out=self.gmod[:, 8:16], in0=self.modc[:, 32:40], scalar=1.0, in1=colv[:, C_N2G:C_N2G + 8],
                                                     op0=ALU.add, op1=ALU.mult), W=[bmodc])
            self.bmod = bmodc
            for which, dst in ((16, self.g1bc), (40, self.g2bc)):
                def mk(which=which):
                    last = None
                    for c in range(8):
                        last = V.tensor_scalar(out=diag[:, c, :], in0=self.idf[:], scalar1=self.modc[:, which + c:which + c + 1], scalar2=None,
                                               op0=ALU.mult)
                    return last
                op("dve", mk, SR=[bmodc], W=[bdiag])

                def mmb():
                    last = None
                    for c in range(8):
                        last = T.matmul(bcps[:, c // 4, (c % 4) * 128:(c % 4 + 1) * 128], self.onesf[:], diag[:, c, :], start=True, stop=True)
                    return last
                op("pe", mmb, R=[bdiag], W=[bbc])
                op("act", lambda dst=dst: A.activation(out=dst[:], in_=bcps[:].rearrange("p a b -> p (a b)"), func=AF.Copy), R=[bbc], W=[bmodc])
            def mk2a():
                last = None
                for c in range(8):
                    last = V.tensor_scalar(out=wtmp[:, c, :], in0=self.wr[:, c, :], scalar1=self.modc[:, 24 + c:25 + c], scalar2=None, op0=ALU.mult)
                return last

            def mk2b():
                last = None
                for c in range(8):
                    last = V.tensor_scalar(out=self.wr[:, c, :], in0=self.wr[:, c, :], scalar1=self.gmod[:, 8 + c:9 + c], scalar2=None, op0=ALU.mult)
                return last
            bwr = Buf()
            op("dve", mk2a, SR=[bmodc], R=[bC, bwr], W=[bdiag])
            op("dve", mk2b, SR=[bmodc], R=[bC], W=[bwr])
            op("dve", lambda: V.tensor_copy(out=self.wrh[:], in_=self.wr[:]), W=[bwr])
            op("dve", lambda: V.tensor_tensor(out=self.wrl[:], in0=self.wr[:], in1=self.wrh[:], op=ALU.subtract), W=[bwr])

            def mmr():
                last = None
                for c in range(8):
                    last = T.matmul(modps[:, 64:128], self.onesf[:], wtmp[:, c, :], start=(c == 0), stop=(c == 7))
                return last
            op("pe", mmr, R=[bdiag], W=[bmodps])
            op("dve", lambda: V.tensor_copy(out=self.rbias[:], in_=modps[:, 64:128]), R=[bmodps], W=[bmodc])
            lam_init = 0.8 - 0.6 * math.exp(-0.3 * 0)
            self.lam_init = lam_init

            bl = Buf()
            op("dve", lambda: V.tensor_tensor(out=lt[:, 0:64], in0=rowv[:, R_LAM:R_LAM + 64], in1=rowv[:, R_LAM + 64:R_LAM + 128], op=ALU.mult),
               R=[bC], W=[bl])
            op("dve", lambda: V.tensor_tensor(out=lt[:, 64:128], in0=rowv[:, R_LAM + 128:R_LAM + 192], in1=rowv[:, R_LAM + 192:R_LAM + 256], op=ALU.mult),
               R=[bC], W=[bl])
            op("dve", lambda: V.tensor_reduce(out=self.lam[:, 2:4], in_=lt[:].rearrange("p (a b) -> p a b", b=64), axis=AX.X, op=ALU.add), W=[bl])
            op("act", lambda: A.activation(out=self.lam[:, 2:4], in_=self.lam[:, 2:4], func=AF.Exp), W=[bl])
            op("dve", lambda: V.tensor_tensor(out=self.lam[:, 0:1], in0=self.lam[:, 2:3], in1=self.lam[:, 3:4], op=ALU.subtract), W=[bl])
            op("dve", lambda: V.tensor_scalar(out=self.lam[:, 0:1], in0=self.lam[:, 0:1], scalar1=lam_init, scalar2=None, op0=ALU.add), W=[bl])
            op("dve", lambda: V.tensor_scalar(out=self.lam[:, 1:2], in0=self.lam[:, 0:1], scalar1=-1.0, scalar2=None, op0=ALU.mult), W=[bl])
            self.blam = bl
            bt = Buf()
            for (pos_i, nb, sin_t, cos_t) in ((self.posf_i, 128, self.sinf, self.cosf), (self.poso_i, 32, self.sino, self.coso)):

                op("dve", lambda pos_i=pos_i, nb=nb: V.tensor_copy(out=posf[:, 0:nb], in_=pos_i[:]), R=[bC], W=[bt])
                op("dve", lambda nb=nb: V.tensor_tensor(out=tmpf[:, 0:nb, :], in0=posf[:, 0:nb].unsqueeze(2).to_broadcast([128, nb, 8]),
                                                        in1=colv[:, C_INVF:C_INVF + 8].unsqueeze(1).to_broadcast([128, nb, 8]), op=ALU.mult),
                   R=[bC], W=[bt])
                for shift, dst in ((0.0, sin_t), (0.5 * math.pi, cos_t)):
                    tk, tf2, ti = tmpk[:, 0:nb, :], tmpf2[:, 0:nb, :], tmpi[:, 0:nb, :]
                    tf = tmpf[:, 0:nb, :]
                    op("dve", lambda tk=tk, tf=tf, shift=shift: V.tensor_scalar(out=tk, in0=tf, scalar1=shift, scalar2=1.0 / TWO_PI, op0=ALU.add, op1=ALU.mult), W=[bt])
                    op("dve", lambda tk=tk, ti=ti: V.tensor_copy(out=ti, in_=tk), W=[bt])
                    op("dve", lambda tk=tk, ti=ti: V.tensor_copy(out=tk, in_=ti), W=[bt])
                    op("dve", lambda tk=tk, tf=tf, tf2=tf2: V.scalar_tensor_tensor(out=tf2, in0=tk, scalar=-6.28125, in1=tf, op0=ALU.mult, op1=ALU.add), W=[bt])
                    op("dve", lambda tk=tk, tf2=tf2: V.scalar_tensor_tensor(out=tk, in0=tk, scalar=-(TWO_PI - 6.28125), in1=tf2, op0=ALU.mult, op1=ALU.add), W=[bt])
                    op("dve", lambda tk=tk, shift=shift: V.tensor_scalar(out=tk, in0=tk, scalar1=shift, scalar2=math.pi, op0=ALU.add, op1=ALU.min), W=[bt])
                    op("dve", lambda tk=tk: V.tensor_scalar(out=tk, in0=tk, scalar1=-math.pi, scalar2=None, op0=ALU.max), W=[bt])
                    op("act", lambda dst=dst, tk=tk: A.activation(out=dst[:], in_=tk, func=AF.Sin), R=[bt], W=[bC])
            self.barrier()
            if "modc" in self.dbg:
                self.dump("modc", self.modc[:]); self.dump("g1bc", self.g1bc[:]); self.dump("rbias", self.rbias[:])
                self.dump("lam", self.lam[:]); self.dump("sinf", self.sinf[:]); self.dump("coso", self.coso[:])
                self.dump("wr", self.wr[:]); self.dump("gmod", self.gmod[:])
                self.barrier()

    def rms_stats(self, x_ap, rows, junk, ss, sd, rstd, bx, bst):
        nc = self.nc
        A, V = nc.scalar, nc.vector

        self.op("act", lambda: A.activation(out=junk[0:rows, :], in_=x_ap, func=AF.Square, accum_out=ss[0:rows, :]), R=[bx], W=[bst])
        self.op("act", lambda: A.activation(out=sd[0:rows, :], in_=ss[0:rows, :], func=AF.Sqrt, scale=1.0 / D, bias=self.epsc[0:rows, :]), W=[bst])
        self.op("dve", lambda: V.reciprocal(out=rstd[0:rows, :], in_=sd[0:rows, :]), W=[bst])

    def qk_process(self, src, bsrc, gcol, cos_ap, sin_ap, kb, bkb, tmp):
        nc = self.nc
        A, V = nc.scalar, nc.vector
        sq, kn, ssk, sdk, rk, r1, r2 = tmp["sq"], tmp["kn"], tmp["ssk"], tmp["sdk"], tmp["rk"], tmp["r1"], tmp["r2"]
        bt = tmp["b"]
        g3 = lambda t: t[:].rearrange("p (g e) -> p g e", e=64)

        o = self.op
        o("dve", lambda: V.tensor_tensor(out=sq[:], in0=src[:], in1=src[:], op=ALU.mult), R=[bsrc], W=[bt])
        o("dve", lambda: V.tensor_reduce(out=ssk[:], in_=g3(sq), axis=AX.X, op=ALU.add), W=[bt])
        o("act", lambda: A.activation(out=sdk[:], in_=ssk[:], func=AF.Sqrt, scale=1.0 / 64, bias=self.epsc[:]), W=[bt])
        o("dve", lambda: V.reciprocal(out=rk[:], in_=sdk[:]), W=[bt])
        o("dve", lambda: V.tensor_tensor(out=g3(kn), in0=g3(src), in1=rk[:].unsqueeze(2).to_broadcast([128, 16, 64]), op=ALU.mult), R=[bsrc], W=[bt])
        o("dve", lambda: V.tensor_tensor(out=g3(kn), in0=g3(kn), in1=self.rowv[:, gcol:gcol + 64].unsqueeze(1).to_broadcast([128, 16, 64]), op=ALU.mult), W=[bt])
        a_, b_ = g3(kn)[:, :, 0:8], g3(kn)[:, :, 8:16]
        c_ = cos_ap.unsqueeze(1).to_broadcast([128, 16, 8])
        s_ = sin_ap.unsqueeze(1).to_broadcast([128, 16, 8])
        b1, b2 = tmp["b1"], tmp["b2"]
        o("dve", lambda: V.tensor_copy(out=kb[:], in_=kn[:]), R=[bt], W=[bkb])
        o("dve", lambda: V.tensor_tensor(out=r1[:], in0=a_, in1=c_, op=ALU.mult), R=[bt], W=[b1])
        o("dve", lambda: V.tensor_tensor(out=r2[:], in0=b_, in1=s_, op=ALU.mult), R=[bt], W=[b2])
        o("dve", lambda: V.tensor_tensor(out=g3(kb)[:, :, 0:8], in0=r1[:], in1=r2[:], op=ALU.subtract), R=[b1, b2], W=[bkb])
        o("dve", lambda: V.tensor_tensor(out=r1[:], in0=b_, in1=c_, op=ALU.mult), R=[bt], W=[b1])
        o("dve", lambda: V.tensor_tensor(out=r2[:], in0=a_, in1=s_, op=ALU.mult), R=[bt], W=[b2])
        o("dve", lambda: V.tensor_tensor(out=g3(kb)[:, :, 8:16], in0=r1[:], in1=r2[:], op=ALU.add), R=[b1, b2], W=[bkb])

    def qk_tmp(self, st):
        return {"sq": self.sb(st, "sq", [128, D], F32), "kn": self.sb(st, "kn", [128, D], F32),
                "ssk": self.sb(st, "ssk", [128, 16], F32), "sdk": self.sb(st, "sdk", [128, 16], F32),
                "rk": self.sb(st, "rk", [128, 16], F32), "r1": self.sb(st, "r1", [128, 16, 8], F32),
                "r2": self.sb(st, "r2", [128, 16, 8], F32), "b": Buf("qktmp"), "b1": Buf(), "b2": Buf()}

    def phase1(self):
        nc = self.nc
        V, A, P, T = nc.vector, nc.scalar, nc.gpsimd, nc.tensor
        op, dma = self.op, self.dma
        NTL = self.cfg.get("n_p1_tiles", 32)
        with ExitStack() as ph:
            wkv = self.sb(ph, "wkv", [128, 4, 8, 512], BF16)
            xt = [self.sb(ph, "xt%d" % k, [128, 4, D], F32) for k in range(2)]
            xn = [self.sb(ph, "xn%d" % k, [128, D], BF16) for k in range(2)]
            junk = self.sb(ph, "junk", [128, D], BF16)
            stt = [[self.sb(ph, "st%d_%d" % (k, q), [128, 1], F32) for q in range(3)] for k in range(2)]
            hT = [self.sb(ph, "hT%d" % k, [128, 8, 512], BF16) for k in range(2)]
            kf = [self.sb(ph, "kf%d" % k, [128, D], F32) for k in range(2)]
            kb = [self.sb(ph, "kb%d" % k, [128, D], BF16) for k in range(2)]
            kTs = [self.sb(ph, "kTs%d" % k, [128, 8, 512], BF16) for k in range(2)]
            vst = [self.sb(ph, "vst%d" % k, [128, 8, 4, 128], BF16) for k in range(2)]
            tmp = self.qk_tmp(ph)
            tp = [self.ps(ph, "tp%d" % k, [128, 8, 128], BF16) for k in range(2)]
            kps = self.ps(ph, "kps", [128, D], F32)
            vps = self.ps(ph, "vps", [128, D], F32)
            tpk = self.ps(ph, "tpk", [128, 8, 128], BF16)
            bwkv = Buf()
            bxt, bxn, bst, bhT, bkf, bkb, bkTs, bvst, btp = ([Buf(), Buf()] for _ in range(9))
            bkps, bvps, btpk = Buf(), Buf(), Buf()
            sw = self.new_dsem()
            sx = [self.new_dsem(), self.new_dsem()]
            sk = [self.new_dsem(), self.new_dsem()]
            sv = [self.new_dsem(), self.new_dsem()]
            for pc in range(4):
                dma("sp", sw, wkv[:, pc], self.winb[:, 3072 + pc * 512:3072 + (pc + 1) * 512].rearrange("(k p) n -> p k n", p=128),
                    R=[self.bA], W=[bwkv])

            def load(g):
                dma("sp", sx[g % 2], xt[g % 2][:], self.xfull[g * 512:(g + 1) * 512, :].rearrange("(s p) d -> p s d", p=128), W=[bxt[g % 2]])
            load(0)
            NSB = NTL * 4

            def stA(n):
                g, s_ = divmod(n, 4)
                gp, q = g % 2, n % 2
                if s_ == 0 and g + 1 < NTL:
                    load(g + 1)
                ss, sd, rstd = stt[q]
                self.rms_stats(xt[gp][:, s_, :], 128, junk, ss, sd, rstd, bxt[gp], bst[q])
                op("dve", lambda: V.tensor_scalar(out=xn[q][:], in0=xt[gp][:, s_, :], scalar1=rstd[:], scalar2=None, op0=ALU.mult),
                   R=[bxt[gp]], SR=[bst[q]], W=[bxn[q]])

            def stB(n):
                g, s_ = divmod(n, 4)
                gp, q = g % 2, n % 2

                def tr():
                    last = None
                    for c in range(8):
                        last = T.transpose(out=tp[q][:, c, :], in_=xn[q][:, c * 128:(c + 1) * 128], identity=self.idb[:])
                    return last
                op("pe", tr, R=[bxn[q]], W=[btp[q]])

                def ev():
                    last = None
                    for c in range(8):
                        last = A.activation(out=hT[gp][:, c, s_ * 128:(s_ + 1) * 128], in_=tp[q][:, c, :], func=AF.Identity,
                                            scale=self.gmod[:, c:c + 1], bias=self.modc[:, c:c + 1])
                    return last
                op("act", ev, R=[btp[q]], W=[bhT[gp]])

            def stC(n):
                g, s_ = divmod(n, 4)
                gp, q = g % 2, n % 2

                def mmk():
                    last = None
                    for pc in range(2):
                        for k in range(8):
                            last = T.matmul(kps[:, pc * 512:(pc + 1) * 512], hT[gp][:, k, s_ * 128:(s_ + 1) * 128], wkv[:, pc, k, :],
                                            start=(k == 0), stop=(k == 7))
                    return last
                op("pe", mmk, R=[bhT[gp], bwkv], W=[bkps])
                op("act", lambda: A.activation(out=kf[q][:], in_=kps[:], func=AF.Copy), R=[bkps], W=[bkf[q]])

                def mmv():
                    last = None
                    for pc in range(2):
                        for k in range(8):
                            last = T.matmul(vps[:, pc * 512:(pc + 1) * 512], hT[gp][:, k, s_ * 128:(s_ + 1) * 128], wkv[:, 2 + pc, k, :],
                                            start=(k == 0), stop=(k == 7))
                    return last
                op("pe", mmv, R=[bhT[gp], bwkv], W=[bvps])
                op("act", lambda: A.activation(out=vst[gp][:, :, s_, :], in_=vps[:].rearrange("p (h v) -> p h v", v=128), func=AF.Copy),
                   R=[bvps], W=[bvst[gp]])
                self.qk_process(kf[q], bkf[q], R_KG, self.cosf[:, n, :], self.sinf[:, n, :], kb[q], bkb[q], tmp)

            def stD(n):
                g, s_ = divmod(n, 4)
                gp, q = g % 2, n % 2

                def trk():
                    last = None
                    for h in range(8):
                        last = T.transpose(out=tpk[:, h, :], in_=kb[q][:, h * 128:(h + 1) * 128], identity=self.idb[:])
                    return last
                op("pe", trk, R=[bkb[q]], W=[btpk])
                op("act", lambda: A.activation(out=kTs[gp][:, :, s_ * 128:(s_ + 1) * 128], in_=tpk[:], func=AF.Copy), R=[btpk], W=[bkTs[gp]])
                if s_ == 3:
                    dma("sp", sk[gp], self.KT.rearrange("h p t -> p h t")[:, :, g * 512:(g + 1) * 512], kTs[gp][:], R=[bkTs[gp]])
                    dma("sp", sv[gp], self.VS.rearrange("h p b v -> p h b v")[:, :, g * 4:(g + 1) * 4, :], vst[gp][:], R=[bvst[gp]])
            for n in range(NSB + 3):
                if n < NSB:
                    stA(n)
                if 0 <= n - 1 < NSB:
                    stB(n - 1)
                if 0 <= n - 2 < NSB:
                    stC(n - 2)
                if 0 <= n - 3 < NSB:
                    stD(n - 3)
            self.barrier()
            if "KT" in self.dbg:
                self.dump("KT", self.KT[:, :, 0:2048]); self.dump("VS", self.VS[:, :, 0:16, :])
                self.barrier()
        self.p01.close()

    def convert_experts(self):
        NE = self.cfg.get("n_exp", NEXP)
        for e in (list(range(NE - 1)) + [NEXP - 1] if NE < NEXP else list(range(NEXP))):
            self.dma("pool", self.sE, self.wgub[e], self.w_gu[e], W=[self.bE])
            self.dma("pool", self.sE, self.wdnb[e], self.w_dn[e], W=[self.bE])

    def dump(self, name, src):
        sid = self.new_dsem()
        self.dma("sp", sid, self.dbg[name], src)

    def finish(self):
        self.barrier()
        self.root.close()


def host_inputs(inputs, cfg):
    f = lambda k: np.asarray(inputs[k], dtype=np.float32)
    x, c = f("x"), f("c")
    pos = np.asarray(inputs["positions"]).astype(np.int32)
    col = lambda v: np.ascontiguousarray(v.reshape(-1, 128).T)
    rep = lambda v: np.ascontiguousarray(np.broadcast_to(v.reshape(1, -1), (128, v.size)))
    inv_freq = (500000.0 ** (-np.arange(0, 16, 2, dtype=np.float32) / 16)).astype(np.float32)
    w_gu = np.concatenate([f("w_exp_gu")[0], f("w_sh_gu")[0][None]], 0)
    w_dn = np.concatenate([f("w_exp_down")[0], f("w_sh_down")[0][None]], 0)
    shared = {
        "idf": np.eye(128, dtype=np.float32), "w_ada": f("w_ada")[0], "w_in": f("w_in")[0], "w_pw2": f("w_pw2")[0],
        "w_out": f("w_out")[0], "w_router": f("w_router")[0], "w_gu": w_gu, "w_dn": w_dn,
    }
    rowv = np.concatenate([rep(f("q_norm_g")[0]), rep(f("k_norm_g")[0]), rep(f("subln_g")[0]),
                           rep(f("lambda_q1")[0]), rep(f("lambda_k1")[0]), rep(f("lambda_q2")[0]), rep(f("lambda_k2")[0]),
                           rep(f("router_bias")[0])], 1)
    assert rowv.shape == (128, NROW)
    dw = f("conv_dw")[0]
    dwc = np.ascontiguousarray(dw.T.reshape(8, 128, 31).transpose(1, 0, 2).reshape(128, 248))
    maps = []
    kk = np.arange(2048)[:, None]
    qq = np.arange(512)[None, :]
    for core in range(8):
        b, j = core // 4, core % 4
        colv = np.concatenate([col(c[b]), col(f("b_ada")[0]), col(f("norm1_g")[0]), col(f("norm2_g")[0]), col(f("conv_dw_b")[0]),
                               col(f("conv_ln_g")[0]), col(f("conv_ln_b")[0]), col(f("b_pw2")[0]), dwc,
                               np.full((128, 1), 0.0 if j == 0 else 1.0, np.float32), rep(inv_freq), col(f("subln_g")[0])], 1).astype(np.float32)
        assert colv.shape == (128, NCOL)
        xown = np.zeros((8, 542, D), np.float32)
        poso = np.zeros((128, 32), np.int32)
        for i in range(8):
            t0 = 512 * (4 * i + j)
            lo = max(t0 - 30, 0)
            xown[i, 542 - (t0 + 512 - lo):] = x[b, lo:t0 + 512]
            poso[:, i * 4:(i + 1) * 4] = pos[b, t0:t0 + 512].reshape(4, 128).T
        cm = (kk <= 512 * j + qq).astype(np.float32).reshape(16, 128, 512).transpose(1, 0, 2)
        m = dict(shared)
        m.update({"xfull": x[b], "xown": xown, "colv": colv, "rowv": rowv, "posf": np.ascontiguousarray(pos[b].reshape(128, 128).T),
                  "poso": poso, "cmask": np.ascontiguousarray(cm).astype(ml_dtypes.bfloat16)})
        maps.append(m)
    return maps


def _phase23(self):
    nc = self.nc
    V, A, P, T = nc.vector, nc.scalar, nc.gpsimd, nc.tensor
    op, dma = self.op, self.dma
    cfg = self.cfg
    NT = cfg.get("n_own_tiles", 8)
    NH = cfg.get("n_heads", 8)
    colv, rowv = self.colv, self.rowv
    lam_scale = 1.0 - self.lam_init
    with ExitStack() as tl:
        QT = self.sb(tl, "QT", [128, 8, 512], BF16)
        mcT = self.sb(tl, "mcT", [128, 8, 512], BF16)
        sgaT = self.sb(tl, "sgaT", [128, 8, 512], BF16)
        yaT = self.sb(tl, "yaT", [128, 8, 512], BF16)
        bQT, bmc, bsga, bya = Buf(), Buf(), Buf(), Buf()
        sxr = self.new_dsem()
        NWB = 3
        wpc = [self.sb(tl, "wpc%d" % k, [128, 8, 512], BF16) for k in range(NWB)]
        bwp = [Buf() for _ in range(NWB)]
        swp = [self.new_dsem() for _ in range(NWB)]
        wctr = [0]

        def wload(src2d):
            k = wctr[0] % NWB
            wctr[0] += 1
            dma("sp", swp[k], wpc[k][:], src2d.rearrange("(k p) n -> p k n", p=128), R=[self.bA], W=[bwp[k]])
            return wpc[k], bwp[k]

        for i in range(NT):
            with ExitStack() as ph:
                nrm = ExitStack()
                hTo = self.sb(ph, "hTo", [128, 8, 544], BF16)
                stt = [[self.sb(ph, "st%d_%d" % (k, q), [128, 1], F32) for q in range(3)] for k in range(2)]
                tp = [self.ps(nrm, "tp%d" % k, [128, 8, 128], BF16) for k in range(2)]
                xo = self.sb(nrm, "xo", [128, 5, D], F32)
                xn = [self.sb(nrm, "xn%d" % k, [128, D], BF16) for k in range(2)]
                junk = self.sb(nrm, "junk", [128, D], BF16)
                bxo, bhTo, bu, bcv, bmean, bmsq, bvar, brstd, bzT, bpa, bpb = (Buf() for _ in range(11))
                bxn, bst, bsgb, bvsq, btn, bsgc, bqf, bqb, btp, bpg = ([Buf(), Buf()] for _ in range(10))
                sxo = self.new_dsem() if i == 0 else self._sxo
                self._sxo = sxo
                dma("sp", sxo, xo[:, 0:4, :], self.xown[i, 0:512, :].rearrange("(s p) d -> p s d", p=128), W=[bxo])
                dma("sp", sxo, xo[0:30, 4, :], self.xown[i, 512:542, :], W=[bxo])
                for s in range(5):
                    rows = 128 if s < 4 else 30
                    q = s % 2
                    ss, sd, rstd = stt[q]
                    self.rms_stats(xo[0:rows, s, :], rows, junk, ss, sd, rstd, bxo, bst[q])
                    op("dve", lambda: V.tensor_scalar(out=xn[q][0:rows, :], in0=xo[0:rows, s, :], scalar1=rstd[0:rows, :], scalar2=None, op0=ALU.mult),
                       R=[bxo], SR=[bst[q]], W=[bxn[q]])

                    def tr():
                        last = None
                        for c in range(8):
                            last = T.transpose(out=tp[q][:, c, 0:rows], in_=xn[q][0:rows, c * 128:(c + 1) * 128], identity=self.idb[0:rows, 0:rows])
                        return last
                    op("pe", tr, R=[bxn[q]], W=[btp[q]])

                    def ev():
                        last = None
                        for c in range(8):
                            last = A.activation(out=hTo[:, c, s * 128:s * 128 + rows], in_=tp[q][:, c, 0:rows], func=AF.Identity,
                                                scale=self.gmod[:, c:c + 1], bias=self.modc[:, c:c + 1])
                        return last
                    op("act", ev, R=[btp[q]], W=[bhTo])
                self.barrier()
                nrm.close()
                if cfg.get("cut") == 1:
                    self.cut_hit = True
                    return
                u = self.sb(ph, "u", [128, 8, 544], BF16)
                sgb = [self.sb(ph, "sgb%d" % k, [128, 544], F32) for k in range(2)]
                cv = self.sb(ph, "cv", [128, 8, 512], F32)
                vsq = [self.sb(ph, "vsq%d" % k, [128, 512], F32) for k in range(2)]
                mean = self.sb(ph, "mean", [128, 512], F32)
                msq = self.sb(ph, "msq", [128, 512], F32)
                var = self.sb(ph, "var", [128, 512], F32)
                rstdv = self.sb(ph, "rstdv", [128, 512], F32)
                tn = [self.sb(ph, "tn%d" % k, [128, 512], F32) for k in range(2)]
                zT = self.sb(ph, "zT", [128, 8, 512], BF16)
                sgc = [self.sb(ph, "sgc%d" % k, [128, 512], F32) for k in range(2)]
                qf = [self.sb(ph, "qf%d" % k, [128, D], F32) for k in range(2)]
                qb = [self.sb(ph, "qb%d" % k, [128, D], BF16) for k in range(2)]
                tmp = self.qk_tmp(ph)
                pa = self.ps(ph, "pa", [128, 2, 512], F32)
                pb = self.ps(ph, "pb", [128, 2, 512], F32)
                pg = [self.ps(ph, "pg%d" % k, [128, 512], F32) for k in range(2)]
                cvs = ExitStack()
                pcv = [self.ps(cvs, "pcv%d" % k, [128, 512], F32) for k in range(2)]
                dg = [self.sb(cvs, "dg%d" % k, [128, 31, 128], BF16) for k in range(2)]
                bpcv, bdg = [Buf(), Buf()], [Buf(), Buf()]
                wa = wb_ = None
                for cc in range(8):
                    if cc % 4 == 0:
                        wa, bwa = wload(self.winb[:, (cc // 4) * 512:(cc // 4 + 1) * 512])
                        wb_, bwb = wload(self.winb[:, 1024 + (cc // 4) * 512:1024 + (cc // 4 + 1) * 512])
                    co = (cc % 4) * 128

                    def mma(w=wa, pt=pa):
                        last = None
                        for hf in range(2):
                            for k in range(8):
                                last = T.matmul(pt[:, hf, 0:271], w[:, k, co:co + 128], hTo[:, k, hf * 271:(hf + 1) * 271], start=(k == 0), stop=(k == 7))
                        return last
                    op("pe", mma, R=[bhTo, bwa], W=[bpa])
                    op("pe", lambda: mma(wb_, pb), R=[bhTo, bwb], W=[bpb])
                    q = cc % 2
                    op("act", lambda: A.activation(out=sgb[q][:, 0:542].rearrange("p (h n) -> p h n", n=271), in_=pb[:, :, 0:271], func=AF.Sigmoid),
                       R=[bpb], W=[bsgb[q]])
                    bucc = Buf()
                    op("dve", lambda: V.tensor_tensor(out=u[:, cc, 0:542].rearrange("p (h n) -> p h n", n=271), in0=pa[:, :, 0:271],
                                                      in1=sgb[q][:, 0:542].rearrange("p (h n) -> p h n", n=271), op=ALU.mult),
                       R=[bpa, bsgb[q]], W=[bucc])
                    ce, E = "dve", V
                    if i == 0:
                        op(ce, lambda: E.tensor_scalar(out=u[:, cc, 0:30], in0=u[:, cc, 0:30], scalar1=colv[:, C_HALO:C_HALO + 1], scalar2=None, op0=ALU.mult),
                           W=[bucc])
                    bcc = Buf()
                    wofs = C_DW + cc * 31

                    def mkdg():
                        last = None
                        for tap in range(31):
                            last = V.tensor_scalar(out=dg[q][:, tap, :], in0=self.idb[:], scalar1=colv[:, wofs + tap:wofs + tap + 1], scalar2=None, op0=ALU.mult)
                        return last
                    op("dve", mkdg, W=[bdg[q]])

                    def mmcv():
                        last = None
                        for tap in range(31):
                            last = T.matmul(pcv[q][:], dg[q][:, tap, :], u[:, cc, tap:tap + 512], start=(tap == 0), stop=(tap == 30))
                        return last
                    op("pe", mmcv, R=[bdg[q], bucc], W=[bpcv[q]])
                    op("act", lambda: A.activation(out=cv[:, cc, :], in_=pcv[q][:], func=AF.Identity, bias=colv[:, C_DWB + cc:C_DWB + cc + 1]),
                       R=[bpcv[q]], W=[bcc])
                    op("act", lambda: A.activation(out=vsq[q][:], in_=cv[:, cc, :], func=AF.Square), R=[bcc], W=[bvsq[q]])
                    op("pe", lambda: T.matmul(pg[0][:], self.onesf[:], cv[:, cc, :], start=(cc == 0), stop=(cc == 7)), R=[bcc], W=[bpg[0]])
                    op("pe", lambda: T.matmul(pg[1][:], self.onesf[:], vsq[q][:], start=(cc == 0), stop=(cc == 7)), R=[bvsq[q]], W=[bpg[1]])
                    bcv.w = bcc.w
                op("dve", lambda: V.tensor_scalar(out=mean[:], in0=pg[0][:], scalar1=1.0 / D, scalar2=None, op0=ALU.mult), R=[bpg[0]], W=[bmean])
                op("dve", lambda: V.tensor_tensor(out=msq[:], in0=mean[:], in1=mean[:], op=ALU.mult), R=[bmean], W=[bmsq])
                op("dve", lambda: V.scalar_tensor_tensor(out=var[:], in0=pg[1][:], scalar=1.0 / D, in1=msq[:], op0=ALU.mult, op1=ALU.subtract),
                   R=[bpg[1], bmsq], W=[bvar])
                op("act", lambda: A.activation(out=var[:], in_=var[:], func=AF.Sqrt, bias=self.epsc[:]), W=[bvar])
                op("dve", lambda: V.reciprocal(out=rstdv[:], in_=var[:]), R=[bvar], W=[brstd])
                for cc in range(8):
                    q = cc % 2
                    op("dve", lambda: V.tensor_tensor(out=tn[q][:], in0=cv[:, cc, :], in1=mean[:], op=ALU.subtract), R=[bmean], W=[btn[q]])
                    op("dve", lambda: V.tensor_tensor(out=tn[q][:], in0=tn[q][:], in1=rstdv[:], op=ALU.mult), R=[brstd], W=[btn[q]])
                    op("act", lambda: A.activation(out=zT[:, cc, :], in_=tn[q][:], func=AF.Silu, scale=colv[:, C_LNG + cc:C_LNG + cc + 1],
                                                   bias=colv[:, C_LNB + cc:C_LNB + cc + 1]), R=[btn[q]], W=[bzT])
                if cfg.get("cut") == 2:
                    self.barrier()
                    self.cut_hit = True
                    return
                self.barrier()
                cvs.close()
                tp = [self.ps(ph, "tpq%d" % k, [128, 8, 128], BF16) for k in range(2)]
                btp = [Buf(), Buf()]
                wq = [wload(self.winb[:, 2048 + pc * 512:2048 + (pc + 1) * 512]) for pc in range(2)]
                for s in range(4):
                    q = s % 2

                    def mmq():
                        last = None
                        for pc in range(2):
                            for k in range(8):
                                last = T.matmul(pa[:, pc, :], hTo[:, k, 30 + s * 128:30 + (s + 1) * 128], wq[pc][0][:, k, :], start=(k == 0), stop=(k == 7))
                        return last
                    op("pe", mmq, R=[bhTo, wq[0][1], wq[1][1]], W=[bpa])
                    op("act", lambda: A.activation(out=qf[q][:], in_=pa[:].rearrange("p a b -> p (a b)"), func=AF.Copy), R=[bpa], W=[bqf[q]])
                    self.qk_process(qf[q], bqf[q], R_QG, self.coso[:, i * 4 + s, :], self.sino[:, i * 4 + s, :], qb[q], bqb[q], tmp)

                    def trq():
                        last = None
                        for h in range(8):
                            last = T.transpose(out=tp[q][:, h, :], in_=qb[q][:, h * 128:(h + 1) * 128], identity=self.idb[:])
                        return last
                    op("pe", trq, R=[bqb[q]], W=[btp[q]])
                    op("act", lambda: A.activation(out=QT[:, :, s * 128:(s + 1) * 128], in_=tp[q][:], func=AF.Copy), R=[btp[q]], W=[bQT])
                for gsel, base in ((0, 5120), (1, 6144)):
                    for pc in range(2):
                        wg, bwg = wload(self.winb[:, base + pc * 512:base + (pc + 1) * 512])
                        for c4 in range(4):
                            dc = pc * 4 + c4
                            q = dc % 2

                            def mmg():
                                last = None
                                for k in range(8):
                                    last = T.matmul(pg[q][:], wg[:, k, c4 * 128:(c4 + 1) * 128], hTo[:, k, 30:542], start=(k == 0), stop=(k == 7))
                                return last
                            op("pe", mmg, R=[bhTo, bwg], W=[bpg[q]])
                            if gsel == 0:
                                op("act", lambda: A.activation(out=sgc[q][:], in_=pg[q][:], func=AF.Sigmoid), R=[bpg[q]], W=[bsgc[q]])
                                op("dve", lambda: V.tensor_copy(out=mcT[:, dc, :], in_=sgc[q][:]), R=[bsgc[q]], W=[bmc])
                            else:
                                op("act", lambda: A.activation(out=sgaT[:, dc, :], in_=pg[q][:], func=AF.Sigmoid), R=[bpg[q]], W=[bsga])
                for pc in range(2):
                    wp, bwpp = wload(self.wpw2b[:, pc * 512:(pc + 1) * 512])
                    for c4 in range(4):
                        dc = pc * 4 + c4
                        q = dc % 2

                        def mmp():
                            last = None
                            for k in range(8):
                                last = T.matmul(pg[q][:], wp[:, k, c4 * 128:(c4 + 1) * 128], zT[:, k, :], start=(k == 0), stop=(k == 7))
                            return last
                        op("pe", mmp, R=[bzT, bwpp], W=[bpg[q]])
                        op("dve", lambda: V.scalar_tensor_tensor(out=mcT[:, dc, :], in0=pg[q][:], scalar=colv[:, C_BPW2 + dc:C_BPW2 + dc + 1],
                                                                 in1=mcT[:, dc, :], op0=ALU.add, op1=ALU.mult), R=[bpg[q]], W=[bmc])
                if i == 0 and "u" in self.dbg:
                    self.barrier()
                    self.dump("u", u[:, :, 0:542]); self.dump("cv", cv[:]); self.dump("zT", zT[:]); self.dump("QT", QT[:])
                    self.dump("mcT", mcT[:]); self.dump("sgaT", sgaT[:]); self.dump("hTo", hTo[:, :, 0:542])
                self.barrier()
            if cfg.get("cut") == 3:
                self.cut_hit = True
                return
            with ExitStack() as ph:
                NKB = 16 * (i + 1)
                NCH = NKB // 16
                kch = [self.sb(ph, "kch%d" % k, [128, 2048], BF16) for k in range(3)]
                vch = [self.sb(ph, "vch%d" % k, [128, 16, 128], BF16) for k in range(3)]
                bkc = [Buf() for _ in range(3)]
                skc = getattr(self, "_skc", None) or [self.new_dsem() for _ in range(3)]
                self._skc = skc
                et = [self.sb(ph, "et%d" % k, [128, 2, 512], BF16) for k in range(3)]
                bet = [Buf() for k in range(3)]
                sps = [self.ps(ph, "sps%d" % k, [128, 2, 512], F32) for k in range(2)]
                bsp = [Buf() for k in range(2)]
                Esum2 = [self.sb(ph, "Esum%d" % k, [128, 2, 512], F32) for k in range(2)]
                bEs2 = [Buf(), Buf()]
                oT = [self.ps(ph, "oT%d" % m, [128, 512], F32) for m in range(2)]
                rs = [self.ps(ph, "rs%d" % m, [128, 512], F32) for m in range(2)]
                boT, brs = [Buf(), Buf()], [Buf(), Buf()]
                R1 = self.sb(ph, "R1", [128, 512], F32)
                R2 = self.sb(ph, "R2", [128, 512], F32)
                Of = self.sb(ph, "Of", [128, 512], F32)
                O2 = self.sb(ph, "O2", [128, 512], F32)
                Osq = self.sb(ph, "Osq", [128, 512], F32)
                sgs = self.sb(ph, "sgs", [128, 1], F32)
                bR1, bR2, bOf, bO2, bOsq, bsgs = (Buf() for _ in range(6))
                op("dve", lambda: V.tensor_scalar(out=sgs[:], in0=colv[:, C_SUBLN:C_SUBLN + 1], scalar1=lam_scale, scalar2=None, op0=ALU.mult), W=[bsgs])

                def kvload(h, ch):
                    slot = ch_index[(h, ch)] % 3
                    dma("sp", skc[slot], kch[slot][:], self.KT[h, :, ch * 2048:(ch + 1) * 2048], W=[bkc[slot]])
                    dma("sp", skc[slot], vch[slot][:], self.VS[h, :, ch * 16:(ch + 1) * 16, :], W=[bkc[slot]])
                chunks = [(h, ch) for h in range(NH) for ch in range(NCH)]
                ch_index = {c: k for k, c in enumerate(chunks)}
                for c in chunks[:3]:
                    kvload(*c)
                steps = [(h, ch, kb) for (h, ch) in chunks for kb in range(16)]

                def qk(n):
                    h, ch, kb = steps[n]
                    slot = ch_index[(h, ch)] % 3
                    first = (ch == 0 and kb == 0)
                    Esum, bEs = Esum2[h % 2], bEs2[h % 2]

                    def mmqk():
                        last = None
                        for m in range(2):
                            last = T.matmul(sps[n % 2][:, m, :], kch[slot][m * 64:(m + 1) * 64, kb * 128:(kb + 1) * 128],
                                            QT[m * 64:(m + 1) * 64, h, :], start=True, stop=True)
                        return last
                    op("pe", mmqk, R=[bkc[slot], bQT], W=[bsp[n % 2]])
                    op("act", lambda: A.activation(out=et[n % 3][:], in_=sps[n % 2][:], func=AF.Exp, scale=0.125), R=[bsp[n % 2]], W=[bet[n % 3]])
                    if ch == NCH - 1:
                        op("dve", lambda: V.tensor_tensor(out=et[n % 3][:], in0=et[n % 3][:],
                                                          in1=self.cmask[:, kb, :].unsqueeze(1).to_broadcast([128, 2, 512]), op=ALU.mult), W=[bet[n % 3]])
                    if first:
                        op("dve", lambda: V.tensor_copy(out=Esum[:, 0, :], in_=et[n % 3][:, 0, :]), R=[bet[n % 3]], W=[bEs])
                    else:
                        op("dve", lambda: V.tensor_tensor(out=Esum[:, 0, :], in0=Esum[:, 0, :], in1=et[n % 3][:, 0, :], op=ALU.add), R=[bet[n % 3]], W=[bEs])

                def pv(n):
                    h, ch, kb = steps[n]
                    slot = ch_index[(h, ch)] % 3
                    first, last = (ch == 0 and kb == 0), (ch == NCH - 1 and kb == 15)
                    Esum, bEs = Esum2[h % 2], bEs2[h % 2]

                    def mmpv():
                        l_ = None
                        for m in range(2):
                            l_ = T.matmul(oT[m][:], vch[slot][:, kb, :], et[n % 3][:, m, :], start=first, stop=last)
                        return l_
                    op("pe", mmpv, R=[bet[n % 3], bkc[slot]], W=[boT[0], boT[1]])
                    op("pe", lambda: T.matmul(rs[1][:], self.onesb[:], et[n % 3][:, 1, :], start=first, stop=last), R=[bet[n % 3]], W=[brs[1]])
                    if last:
                        op("pe", lambda: T.matmul(rs[0][:], self.onesf[:], Esum[:, 0, :], start=True, stop=True), R=[bEs], W=[brs[0]])
                    if kb == 15 and ch_index[(h, ch)] + 3 < len(chunks):
                        kvload(*chunks[ch_index[(h, ch)] + 3])
                    if last:
                        op("dve", lambda: V.reciprocal(out=R1[:], in_=rs[0][:]), R=[brs[0]], W=[bR1])
                        op("dve", lambda: V.reciprocal(out=R2[:], in_=rs[1][:]), R=[brs[1]], W=[bR2])
                        op("dve", lambda: V.tensor_scalar(out=R2[:], in0=R2[:], scalar1=self.lam[:, 1:2], scalar2=None, op0=ALU.mult), W=[bR2])
                        op("dve", lambda: V.tensor_tensor(out=Of[:], in0=oT[0][:], in1=R1[:], op=ALU.mult), R=[boT[0], bR1], W=[bOf])
                        op("dve", lambda: V.tensor_tensor(out=O2[:], in0=oT[1][:], in1=R2[:], op=ALU.mult), R=[boT[1], bR2], W=[bO2])
                        op("dve", lambda: V.tensor_tensor(out=Of[:], in0=Of[:], in1=O2[:], op=ALU.add), R=[bO2], W=[bOf])
                        op("act", lambda: A.activation(out=Osq[:], in_=Of[:], func=AF.Square), R=[bOf], W=[bOsq])
                        op("pe", lambda: T.matmul(rs[0][:], self.onesf[:], Osq[:], start=True, stop=True), R=[bOsq], W=[brs[0]])
                        op("act", lambda: A.activation(out=R1[:], in_=rs[0][:], func=AF.Sqrt, scale=1.0 / 128, bias=self.epsc[:]), R=[brs[0]], W=[bR1])
                        op("dve", lambda: V.reciprocal(out=R1[:], in_=R1[:]), W=[bR1])
                        op("dve", lambda: V.scalar_tensor_tensor(out=yaT[:, h, :], in0=Of[:], scalar=sgs[:, 0:1], in1=R1[:], op0=ALU.mult, op1=ALU.mult),
                           R=[bOf, bR1], SR=[bsgs], W=[bya])
                for n in range(len(steps) + 1):
                    if n < len(steps):
                        qk(n)
                    if n >= 1:
                        pv(n - 1)
                if i == 0 and "yaT" in self.dbg:
                    self.barrier()
                    self.dump("yaT", yaT[:])
                self.barrier()
            if cfg.get("cut") == 4:
                self.cut_hit = True
                return
            with ExitStack() as ph:
                mg = self.sb(ph, "mg", [128, 8, 512], BF16)
                xres = self.sb(ph, "xres", [128, 4, D], F32)
                bxres = Buf()
                dma("sp", sxr, xres[:], self.xown[i, 30:542, :].rearrange("(s p) d -> p s d", p=128), W=[bxres])
                x1 = self.sb(ph, "x1", [128, 4, D], F32)
                t1 = self.sb(ph, "t1", [128, D], F32)
                junk = self.sb(ph, "junk", [128, D], BF16)
                stt = [[self.sb(ph, "st%d_%d" % (k, q), [128, 1], F32) for q in range(3)] for k in range(2)]
                xn2 = [self.sb(ph, "xn2_%d" % k, [128, D], F32) for k in range(2)]
                xT = [self.sb(ph, "xT%d" % k, [128, 8, 128], BF16) for k in range(2)]
                xL = [self.sb(ph, "xL%d" % k, [128, 8, 128], BF16) for k in range(2)]
                h2st = self.sb(ph, "h2st", [128, 8, 512], BF16)
                gst = self.sb(ph, "gst", [64, 512], F32)
                R_ = {k: self.sb(ph, "r_" + k, [128, 64], F32) for k in ("lg", "sc", "ch", "eq", "cm", "sel", "w")}
                Rs = {k: self.sb(ph, "rs_" + k, [128, 8], F32) for k in ("m1", "m2", "gs", "t8", "gm", "u8", "sm")}
                bR = {k: Buf() for k in list(R_) + list(Rs)}
                plg = self.ps(ph, "plg", [128, 512], F32)
                po = self.ps(ph, "po", [128, 2, 512], F32)
                tph = self.ps(ph, "tph", [128, 8, 128], BF16)
                tpl = self.ps(ph, "tpl", [128, 8, 128], BF16)
                xhi = [self.sb(ph, "xhi%d" % k, [128, D], BF16) for k in range(2)]
                xlo = [self.sb(ph, "xlo%d" % k, [128, D], BF16) for k in range(2)]
                bxhi, bxlo = [Buf(), Buf()], [Buf(), Buf()]
                btph, btpl = Buf(), Buf()
                bmg, bx1, bt1, bh2, bgst, bpo, bptf, bplg = (Buf() for _ in range(8))
                bst, bxn2, bxT = ([Buf(), Buf()] for _ in range(3))
                sst = getattr(self, "_sst", None) or [self.new_dsem() for _ in range(3)]
                self._sst = sst
                for dc in range(8):
                    e_, E = "dve", V
                    bm_ = Buf()
                    op(e_, lambda: E.tensor_tensor(out=mg[:, dc, :], in0=sgaT[:, dc, :], in1=yaT[:, dc, :], op=ALU.mult), R=[bsga, bya], W=[bm_])
                    op(e_, lambda: E.tensor_tensor(out=mg[:, dc, :], in0=mg[:, dc, :], in1=mcT[:, dc, :], op=ALU.add), R=[bmc], W=[bm_])
                    bmg.r[("x", dc)] = bm_.w
                wo = [wload(self.woutb[:, pc * 512:(pc + 1) * 512]) for pc in range(2)]
                if cfg.get("xtra"):
                    for _ in range(cfg["xtra"]):
                        op("pe", lambda: T.matmul(plg[:, 0:128], self.idb[:], self.idb[:], start=True, stop=True), W=[bplg])
                def X2c(s):
                    q = s % 2

                    def mmo():
                        last = None
                        for pc in range(2):
                            for k in range(8):
                                last = T.matmul(po[:, pc, :], mg[:, k, s * 128:(s + 1) * 128], wo[pc][0][:, k, :], start=(k == 0), stop=(k == 7))
                        return last
                    self._pre("pe", [], [bmg], [])
                    op("pe", mmo, R=[wo[0][1], wo[1][1]], W=[bpo])
                    op("dve", lambda: V.tensor_tensor(out=t1[:], in0=po[:].rearrange("p a b -> p (a b)"), in1=self.g1bc[:], op=ALU.mult), R=[bpo], W=[bt1])
                    bx1s = Buf()
                    op("dve", lambda: V.tensor_tensor(out=x1[:, s, :], in0=t1[:], in1=xres[:, s, :], op=ALU.add), R=[bt1, bxres], W=[bx1s])
                    bx1.r[("x", s)] = bx1s.w
                    if cfg.get("cut") == 5:
                        return
                    ss, sd, rstd = stt[q]
                    self.rms_stats(x1[:, s, :], 128, junk, ss, sd, rstd, bx1s, bst[q])
                    op("dve", lambda: V.tensor_scalar(out=xn2[q][:], in0=x1[:, s, :], scalar1=rstd[:], scalar2=None, op0=ALU.mult),
                       R=[bx1s], SR=[bst[q]], W=[bxn2[q]])

                    if cfg.get("cut") == 61:
                        return
                    op("act", lambda: A.activation(out=xhi[q][:], in_=xn2[q][:], func=AF.Copy), R=[bxn2[q]], W=[bxhi[q]])
                    op("dve", lambda: V.tensor_tensor(out=xlo[q][:], in0=xn2[q][:], in1=xhi[q][:], op=ALU.subtract), R=[bxn2[q], bxhi[q]], W=[bxlo[q]])

                    def trh():
                        last = None
                        for c in range(8):
                            last = T.transpose(out=tph[:, c, :], in_=xhi[q][:, c * 128:(c + 1) * 128], identity=self.idb[:])
                        return last
                    op("pe", trh, R=[bxhi[q]], W=[btph])

                    def evh():
                        last = None
                        for c in range(8):
                            last = A.activation(out=h2st[:, c, s * 128:(s + 1) * 128], in_=tph[:, c, :], func=AF.Identity,
                                                scale=self.gmod[:, 8 + c:9 + c], bias=self.modc[:, 24 + c:25 + c])
                        return last
                    op("act", evh, R=[btph], W=[bh2])
                    op("act", lambda: A.activation(out=xT[q][:], in_=tph[:], func=AF.Copy), R=[btph], W=[bxT[q]])

                    def trl():
                        last = None
                        for c in range(8):
                            last = T.transpose(out=tpl[:, c, :], in_=xlo[q][:, c * 128:(c + 1) * 128], identity=self.idb[:])
                        return last
                    op("pe", trl, R=[bxlo[q]], W=[btpl])
                    op("act", lambda: A.activation(out=xL[q][:], in_=tpl[:], func=AF.Copy), R=[btpl], W=[bxT[q]])
                    if cfg.get("cut") == 63:
                        return

                def Y2c(s):
                    q = s % 2

                    def mmr():
                        last = None
                        for pi_, (xa, wa_) in enumerate(((xT[q], self.wrh), (xL[q], self.wrh), (xT[q], self.wrl))):
                            for c in range(8):
                                last = T.matmul(plg[:, 0:64], xa[:, c, :], wa_[:, c, :], start=(pi_ == 0 and c == 0), stop=(pi_ == 2 and c == 7))
                        return last
                    if cfg.get("cut") == 65:
                        op("pe", lambda: T.matmul(po[:, 1, :], self.idb[:], mg[:, 0, :], start=True, stop=True), W=[bpo])
                        return
                    if cfg.get("cut") == 66:
                        if s == 3:
                            op("pe", mmr, R=[bxT[q]], W=[bplg])
                        return
                    op("pe", mmr, R=[bxT[q]], W=[bplg])
                    if cfg.get("cut") == 6:
                        return
                    r3 = lambda t: t[:].rearrange("p (g e) -> p g e", e=8)
                    lg, sc, chh, eq, cm, sel, w_ = (R_[k] for k in ("lg", "sc", "ch", "eq", "cm", "sel", "w"))
                    m1, m2, gs, t8, gm, u8, sm = (Rs[k] for k in ("m1", "m2", "gs", "t8", "gm", "u8", "sm"))
                    B_ = bR
                    op("dve", lambda: V.tensor_tensor(out=lg[:], in0=plg[:, 0:64], in1=self.rbias[:], op=ALU.add), R=[bplg], W=[B_["lg"]])
                    op("act", lambda: A.activation(out=sc[:], in_=lg[:], func=AF.Sigmoid), R=[B_["lg"]], W=[B_["sc"]])
                    op("dve", lambda: V.tensor_tensor(out=chh[:], in0=sc[:], in1=rowv[:, R_RB:R_RB + 64], op=ALU.add), R=[B_["sc"]], W=[B_["ch"]])
                    op("dve", lambda: V.tensor_reduce(out=m1[:], in_=r3(chh), axis=AX.X, op=ALU.max), R=[B_["ch"]], W=[B_["m1"]])
                    op("dve", lambda: V.tensor_tensor(out=r3(eq), in0=r3(chh), in1=m1[:].unsqueeze(2).to_broadcast([128, 8, 8]), op=ALU.is_equal),
                       R=[B_["ch"], B_["m1"]], W=[B_["eq"]])
                    op("dve", lambda: V.scalar_tensor_tensor(out=eq[:], in0=eq[:], scalar=-1e4, in1=chh[:], op0=ALU.mult, op1=ALU.add), R=[B_["ch"]], W=[B_["eq"]])
                    op("dve", lambda: V.tensor_reduce(out=m2[:], in_=r3(eq), axis=AX.X, op=ALU.max), R=[B_["eq"]], W=[B_["m2"]])
                    op("dve", lambda: V.tensor_tensor(out=gs[:], in0=m1[:], in1=m2[:], op=ALU.add), R=[B_["m1"], B_["m2"]], W=[B_["gs"]])
                    op("dve", lambda: V.max(out=t8[:], in_=gs[:]), R=[B_["gs"]], W=[B_["t8"]])
                    op("dve", lambda: V.tensor_scalar(out=gm[:], in0=gs[:], scalar1=t8[:, 3:4], scalar2=None, op0=ALU.is_ge), R=[B_["gs"]], SR=[B_["t8"]], W=[B_["gm"]])
                    op("dve", lambda: V.tensor_scalar(out=gm[:], in0=gm[:], scalar1=-1.0, scalar2=1e4, op0=ALU.add, op1=ALU.mult), W=[B_["gm"]])
                    op("dve", lambda: V.tensor_tensor(out=r3(cm), in0=r3(chh), in1=gm[:].unsqueeze(2).to_broadcast([128, 8, 8]), op=ALU.add),
                       R=[B_["ch"], B_["gm"]], W=[B_["cm"]])
                    op("dve", lambda: V.max(out=u8[:], in_=cm[:]), R=[B_["cm"]], W=[B_["u8"]])
                    op("dve", lambda: V.tensor_scalar(out=sel[:], in0=cm[:], scalar1=u8[:, 7:8], scalar2=None, op0=ALU.is_ge), R=[B_["cm"]], SR=[B_["u8"]], W=[B_["sel"]])
                    op("dve", lambda: V.tensor_tensor(out=w_[:], in0=sc[:], in1=sel[:], op=ALU.mult), R=[B_["sc"], B_["sel"]], W=[B_["w"]])
                    op("dve", lambda: V.tensor_reduce(out=sm[:, 0:1], in_=w_[:], axis=AX.X, op=ALU.add), R=[B_["w"]], W=[B_["sm"]])
                    op("dve", lambda: V.reciprocal(out=sm[:, 1:2], in_=sm[:, 0:1]), W=[B_["sm"]])
                    op("dve", lambda: V.tensor_scalar(out=w_[:], in0=w_[:], scalar1=sm[:, 1:2], scalar2=2.5, op0=ALU.mult, op1=ALU.mult), SR=[B_["sm"]], W=[B_["w"]])
                    op("pe", lambda: T.transpose(out=plg[0:64, 128:256], in_=w_[:], identity=self.idf[:]), R=[B_["w"]], W=[bplg])
                    op("act", lambda: A.activation(out=gst[:, s * 128:(s + 1) * 128], in_=plg[0:64, 128:256], func=AF.Copy), R=[bplg], W=[bgst])
                for n in range(5):
                    if n < 4:
                        X2c(n)
                    if n >= 1 and not (cfg.get("cut") in (5, 61, 62, 63)):
                        Y2c(n - 1)
                if cfg.get("cut") in (5, 6, 61, 62, 63, 65, 66):
                    self.barrier()
                    self.dump("x1", x1[:])
                    if cfg.get("cut") == 64:
                        self.dump("h2st", h2st[:])
                    self.barrier()
                    self.cut_hit = True
                    return
                self._pre("sp", [], [bx1], [])
                dma("sp", sst[0], self.x1s[i * 512:(i + 1) * 512, :].rearrange("(s p) d -> p s d", p=128), x1[:], R=[bx1])
                dma("sp", sst[1], self.h2Ts[:, :, i * 512:(i + 1) * 512], h2st[:], R=[bh2])
                dma("sp", sst[2], self.gTs[0:64, i * 512:(i + 1) * 512], gst[:], R=[bgst])
                if i == 0 and "x1" in self.dbg:
                    self.barrier()
                    self.dump("x1", x1[:]); self.dump("h2st", h2st[:]); self.dump("gst", gst[:])
                self.barrier()


Builder.phase23 = _phase23


def _phase4(self):
    nc = self.nc
    V, A, P, T = nc.vector, nc.scalar, nc.gpsimd, nc.tensor
    op, dma = self.op, self.dma
    cfg = self.cfg
    NQ = cfg.get("n_quarters", 4)
    NE = cfg.get("n_exp", NEXP)
    elist = list(range(NE - 1)) + [NEXP - 1] if NE < NEXP else list(range(NEXP))
    with ExitStack() as ph:
        h2q = self.sb(ph, "h2q", [128, 8, 1024], BF16)
        acc = self.sb(ph, "acc", [128, 8, D], F32)
        NEB = 3
        wgu = [self.sb(ph, "wgu%d" % k, [128, 8, 512], BF16) for k in range(NEB)]
        wdn = [self.sb(ph, "wdn%d" % k, [128, 2, D], BF16) for k in range(NEB)]
        bwe = [Buf() for _ in range(NEB)]
        swe = [self.new_dsem() for _ in range(NEB)]
        gb = [self.sb(ph, "gb%d" % k, [128, 1024], F32) for k in range(2)]
        bgb = [Buf(), Buf()]
        sgb = [self.new_dsem(), self.new_dsem()]
        sg = [self.sb(ph, "sg%d" % k, [128, 512], F32) for k in range(2)]
        tt = [self.sb(ph, "tt%d" % k, [128, 512], F32) for k in range(2)]
        act = [self.sb(ph, "act%d" % k, [128, 2, 512], BF16) for k in range(2)]
        xr = [self.sb(ph, "xr%d" % k, [128, D], F32) for k in range(2)]
        ot = [self.sb(ph, "ot%d" % k, [128, D], F32) for k in range(2)]
        bsg, btt, bact, bxr, bot = ([Buf(), Buf()] for _ in range(5))
        sxr = [self.new_dsem(), self.new_dsem()]
        sot = [self.new_dsem(), self.new_dsem()]
        sh2 = self.new_dsem()
        pgu = [self.ps(ph, "pgu%d" % k, [128, 2, 512], F32) for k in range(2)]
        pd = [self.ps(ph, "pd%d" % k, [128, 2, 512], F32) for k in range(2)]
        bpgu, bpd = [Buf(), Buf()], [Buf(), Buf()]
        bh2q, bacc = Buf(), [Buf() for _ in range(8)]

        def wl(n, e):
            k = n % NEB
            dma("sp", swe[k], wgu[k][:], self.wgub[e].rearrange("(k p) n -> p k n", p=128), R=[self.bE], W=[bwe[k]])
            dma("sp", swe[k], wdn[k][:], self.wdnb[e].rearrange("(c p) n -> p c n", p=128), R=[self.bE], W=[bwe[k]])

        def gl(n, e, qt):
            k = n % 2
            if e < 64:
                dma("sp", sgb[k], gb[k][:], self.gTs[e:e + 1, qt * 1024:(qt + 1) * 1024].partition_broadcast(128), W=[bgb[k]])
        NT = cfg.get("n_own_tiles", 8)
        if NT < 2 * NQ:
            bz = Buf()
            op("dve", lambda: V.memset(acc[:], 0.0), W=[bz])
            op("dve", lambda: V.memset(h2q[:], 0.0), W=[bz])
            sz = self.new_dsem()
            for i in range(NT, 2 * NQ):
                dma("sp", sz, self.h2Ts[:, :, i * 512:(i + 1) * 512], h2q[:, :, 0:512], R=[bz])
                dma("sp", sz, self.gTs[0:64, i * 512:(i + 1) * 512], acc[0:64, 0, 0:512], R=[bz])
                dma("sp", sz, self.x1s[i * 512:(i + 1) * 512, :].rearrange("(s p) d -> p s d", p=128), acc[:, 0:4, :], R=[bz])
            self.barrier()
        n = 0
        for qt in range(NQ):
            dma("sp", sh2, h2q[:], self.h2Ts[:, :, qt * 1024:(qt + 1) * 1024], W=[bh2q])
            n0 = n
            wl(n, elist[0])
            gl(n, elist[0], qt)
            if len(elist) > 1:
                wl(n + 1, elist[1])
            pend = None
            for ei, e in enumerate(elist):
                if ei + 1 < len(elist):
                    gl(n + 1, elist[ei + 1], qt)
                wk, gk = n % NEB, n % 2
                for t in range(2):
                    ak = (2 * n + t) % 2
                    for c in range(2):
                        pk = (2 * (2 * n + t) + c) % 2

                        def mmgu():
                            last = None
                            for half in range(2):
                                for k in range(8):
                                    last = T.matmul(pgu[pk][:, half, :], wgu[wk][:, k, half * 256 + c * 128:half * 256 + (c + 1) * 128],
                                                    h2q[:, k, t * 512:(t + 1) * 512], start=(k == 0), stop=(k == 7))
                            return last
                        op("pe", mmgu, R=[bwe[wk], bh2q], W=[bpgu[pk]])
                        op("act", lambda: A.activation(out=sg[pk][:], in_=pgu[pk][:, 0, :], func=AF.Silu), R=[bpgu[pk]], W=[bsg[pk]])
                        op("dve", lambda: V.tensor_tensor(out=tt[pk][:], in0=pgu[pk][:, 1, :], in1=sg[pk][:], op=ALU.mult), R=[bpgu[pk], bsg[pk]], W=[btt[pk]])
                        if e < 64:
                            op("pool", lambda: P.tensor_tensor(out=act[ak][:, c, :], in0=tt[pk][:], in1=gb[gk][:, t * 512:(t + 1) * 512], op=ALU.mult),
                               R=[btt[pk], bgb[gk]], W=[bact[ak]])
                        else:
                            op("pool", lambda: P.tensor_copy(out=act[ak][:, c, :], in_=tt[pk][:]), R=[btt[pk]], W=[bact[ak]])
                    cur = (ak, wk, t, ei == 0)
                    if pend is not None:
                        self._down(pend, act, wdn, pd, bpd, bact, bwe, acc, bacc, T, V)
                    pend = cur
                    if t == 0 and ei + 2 < len(elist):
                        wl(n + 2, elist[ei + 2])
                n += 1
            self._down(pend, act, wdn, pd, bpd, bact, bwe, acc, bacc, T, V)
            for sb_ in range(8):
                k = sb_ % 2
                r0 = qt * 1024 + sb_ * 128
                dma("sp", sxr[k], xr[k][:], self.x1s[r0:r0 + 128, :], W=[bxr[k]])
                op("dve", lambda: V.tensor_tensor(out=ot[k][:], in0=acc[:, sb_, :], in1=self.g2bc[:], op=ALU.mult), R=[bacc[sb_]], W=[bot[k]])
                op("dve", lambda: V.tensor_tensor(out=ot[k][:], in0=ot[k][:], in1=xr[k][:], op=ALU.add), R=[bxr[k]], W=[bot[k]])
                dma("sp", sot[k], self.y[r0:r0 + 128, :], ot[k][:], R=[bot[k]])
            self.barrier()


def _down(self, item, act, wdn, pd, bpd, bact, bwe, acc, bacc, T, V):
    ak, wk, t, first = item
    for s in range(4):
        dk = s % 2

        def mmd():
            last = None
            for pc in range(2):
                for c in range(2):
                    last = T.matmul(pd[dk][:, pc, :], act[ak][:, c, s * 128:(s + 1) * 128], wdn[wk][:, c, pc * 512:(pc + 1) * 512],
                                    start=(c == 0), stop=(c == 1))
            return last
        self.op("pe", mmd, R=[bact[ak], bwe[wk]], W=[bpd[dk]])
        sb_ = t * 4 + s
        src = pd[dk][:].rearrange("p a b -> p (a b)")
        if first:
            self.op("dve", lambda: V.tensor_copy(out=acc[:, sb_, :], in_=src), R=[bpd[dk]], W=[bacc[sb_]])
        else:
            self.op("dve", lambda: V.tensor_tensor(out=acc[:, sb_, :], in0=acc[:, sb_, :], in1=src, op=ALU.add), R=[bpd[dk]], W=[bacc[sb_]])


Builder.phase4 = _phase4
Builder._down = _down


def build(cfg):
    b = Builder(cfg)
    b.declare()
    b.phase0()
    stop = cfg.get("stop", 99)
    if stop >= 1:
        b.phase1()
    b.convert_experts()
    if stop >= 2:
        b.phase23()
    if stop >= 3 and not getattr(b, "cut_hit", False):
        b.phase4()
    b.finish()
    return b


def kernel(**inputs):
    cfg = {}
    b = build(cfg)
    maps = host_inputs(inputs, cfg)
    res = run_bass_kernel_spmd(b.nc, maps, core_ids=list(range(8)))
    out = np.zeros((2, S, D), np.float32)
    for core in range(8):
        bb, j = core // 4, core % 4
        y = np.asarray(res.results[core]["y"], dtype=np.float32)
        for i in range(8):
            t0 = 512 * (4 * i + j)
            out[bb, t0:t0 + 512] = y[i * 512:(i + 1) * 512]
    return out
```

```python
import math
from contextlib import ExitStack
import numpy as np
import ml_dtypes
import concourse.bass as bass
import concourse.mybir as mybir
from concourse.bass_utils import run_bass_kernel_spmd

F32 = mybir.dt.float32
BF16 = mybir.dt.bfloat16
I32 = mybir.dt.int32
AF = mybir.ActivationFunctionType
ALU = mybir.AluOpType
AX = mybir.AxisListType

D = 1024
S = 16384
EPS = 1e-6
NEXP = 65
TWO_PI = 2.0 * math.pi

C_C, C_BADA, C_N1G, C_N2G, C_DWB, C_LNG, C_LNB, C_BPW2, C_DW, C_HALO, C_INVF, C_SUBLN = 0, 8, 56, 64, 72, 80, 88, 96, 104, 352, 353, 361
NCOL = 362
R_QG, R_KG, R_SUBLN, R_LAM, R_RB = 0, 64, 128, 256, 512
NROW = 576


class Buf:
    __slots__ = ("w", "r", "name")

    def __init__(self, name=""):
        self.w = None
        self.r = {}
        self.name = name


class Builder:
    def __init__(self, cfg):
        self.cfg = cfg
        self.nc = bass.Bass("TRN2", target_bir_lowering=False)
        nc = self.nc
        self.root = ExitStack()
        self.eng = {"pe": nc.tensor, "act": nc.scalar, "dve": nc.vector, "pool": nc.gpsimd, "sp": nc.sync}
        self.prog = {e: self.root.enter_context(nc.semaphore("prog_" + e)) for e in ("pe", "act", "dve", "pool")}
        self.cnt = {e: 0 for e in self.prog}
        self.waited = {}
        self.dsems = []
        self.free_dsems = []
        self.uid = 0

    def sig(self, e, ins):
        self.cnt[e] += 1
        ins.then_inc(self.prog[e], 1)
        return ("c", e, self.cnt[e])

    def wait(self, e, tok, force=False):
        if tok is None:
            return
        if tok[0] == "c":
            _, p, c = tok
            if p == e and e == "pe" and not force:
                return
            key = (e, "c", p)
            if self.waited.get(key, 0) >= c:
                return
            self.eng[e].wait_ge(self.prog[p], c)
            self.waited[key] = c
        else:
            _, sid, c = tok
            key = (e, "d", sid)
            if self.waited.get(key, 0) >= c:
                return
            self.eng[e].wait_ge(self.dsems[sid][0], c)
            self.waited[key] = c

    def _pre(self, e, R, W, SR):
        R, W, SR = [getattr(b, "b", b) for b in R], [getattr(b, "b", b) for b in W], [getattr(b, "b", b) for b in SR]
        for b in R:
            self.wait(e, b.w)
        for b in SR:
            self.wait(e, b.w, force=True)
        for b in W:
            self.wait(e, b.w)
            for t in b.r.values():
                self.wait(e, t)

    def _post(self, tok, key, R, W, SR):
        R, W, SR = [getattr(b, "b", b) for b in R], [getattr(b, "b", b) for b in W], [getattr(b, "b", b) for b in SR]
        for b in R:
            b.r[key] = tok
        for b in SR:
            b.r[key] = tok
        for b in W:
            b.w = tok
            b.r = {}

    def op(self, e, fn, R=(), W=(), SR=()):
        self._pre(e, R, W, SR)
        ins = fn()
        tok = self.sig(e, ins)
        self._post(tok, ("c", e), R, W, SR)
        return tok

    def new_dsem(self):
        sem = self.root.enter_context(self.nc.semaphore("dsem%d" % len(self.dsems)))
        self.dsems.append([sem, 0])
        return len(self.dsems) - 1

    def dma(self, q, sid, out, in_, R=(), W=()):
        self._pre(q, R, W, ())
        ins = self.eng[q].dma_start(out=out, in_=in_)
        self.dsems[sid][1] += 16
        ins.then_inc(self.dsems[sid][0], 16)
        tok = ("d", sid, self.dsems[sid][1])
        self._post(tok, ("d", sid), R, W, ())
        return tok

    def barrier(self):
        toks = [("c", e, self.cnt[e]) for e in self.prog if self.cnt[e] > 0]
        toks += [("d", i, d[1]) for i, d in enumerate(self.dsems) if d[1] > 0 and i not in getattr(self, "nobar", ())]
        for e in self.eng:
            for t in toks:
                self.wait(e, t)

    def sb(self, st, name, shape, dt):
        self.uid += 1
        return st.enter_context(self.nc.sbuf_tensor("%s_%d" % (name, self.uid), list(shape), dt))

    def ps(self, st, name, shape, dt):
        self.uid += 1
        return st.enter_context(self.nc.psum_tensor("%s_%d" % (name, self.uid), list(shape), dt))

    def declare(self):
        nc, cfg = self.nc, self.cfg

        def din(name, shape, dt=F32):
            return nc.dram_tensor(name, list(shape), dt, kind="ExternalInput").ap()

        def dscr(name, shape, dt):
            return nc.dram_tensor(name, list(shape), dt).ap()

        self.xfull = din("xfull", [S, D])
        self.xown = din("xown", [8, 542, D])
        self.colv_d = din("colv", [128, NCOL])
        self.rowv_d = din("rowv", [128, NROW])
        self.posf_d = din("posf", [128, 128], I32)
        self.poso_d = din("poso", [128, 32], I32)
        self.cmask_d = din("cmask", [128, 16, 512], BF16)
        self.idf_d = din("idf", [128, 128])
        self.w_ada = din("w_ada", [D, 6 * D])
        self.w_in = din("w_in", [D, 7 * D])
        self.w_pw2 = din("w_pw2", [D, D])
        self.w_out = din("w_out", [D, D])
        self.w_router = din("w_router", [D, 64])
        self.w_gu = din("w_gu", [NEXP, D, 512])
        self.w_dn = din("w_dn", [NEXP, 256, D])
        self.y = nc.dram_tensor("y", [4096, D], F32, kind="ExternalOutput").ap()
        self.winb = dscr("winb", [D, 7 * D], BF16)
        self.wpw2b = dscr("wpw2b", [D, D], BF16)
        self.woutb = dscr("woutb", [D, D], BF16)
        self.wgub = dscr("wgub", [NEXP, D, 512], BF16)
        self.wdnb = dscr("wdnb", [NEXP, 256, D], BF16)
        self.KT = dscr("KT", [8, 128, S], BF16)
        self.VS = dscr("VS", [8, 128, 128, 128], BF16)
        self.x1s = dscr("x1s", [4096, D], F32)
        self.h2Ts = dscr("h2Ts", [128, 8, 4096], BF16)
        self.gTs = dscr("gTs", [NEXP, 4096], F32)
        self.dbg = {}
        for name, shape, dt in cfg.get("debug", []):
            self.dbg[name] = nc.dram_tensor("dbg_" + name, list(shape), dt, kind="ExternalOutput").ap()

    def phase0(self):
        nc, st = self.nc, self.root
        V, A, P, T = nc.vector, nc.scalar, nc.gpsimd, nc.tensor
        op, dma = self.op, self.dma
        sA, sE = self.new_dsem(), self.new_dsem()
        bA, bE = Buf("convA"), Buf("convE")
        for r in range(8):
            dma("pool", sA, self.winb[r * 128:(r + 1) * 128, :], self.w_in[r * 128:(r + 1) * 128, :], W=[bA])
        for r in range(2):
            dma("pool", sA, self.wpw2b[r * 512:(r + 1) * 512, :], self.w_pw2[r * 512:(r + 1) * 512, :], W=[bA])
            dma("pool", sA, self.woutb[r * 512:(r + 1) * 512, :], self.w_out[r * 512:(r + 1) * 512, :], W=[bA])
        self.bA, self.bE, self.sE = bA, bE, sE
        self.nobar = {sE}
        self.colv = self.sb(st, "colv", [128, NCOL], F32)
        self.rowv = self.sb(st, "rowv", [128, NROW], F32)
        self.idf = self.sb(st, "idf", [128, 128], F32)
        self.idb = self.sb(st, "idb", [128, 128], BF16)
        self.onesf = self.sb(st, "onesf", [128, 128], F32)
        self.onesb = self.sb(st, "onesb", [128, 128], BF16)
        self.cmask = self.sb(st, "cmask", [128, 16, 512], BF16)
        self.posf_i = self.sb(st, "posf_i", [128, 128], I32)
        self.poso_i = self.sb(st, "poso_i", [128, 32], I32)
        self.epsc = self.sb(st, "epsc", [128, 1], F32)
        self.modc = self.sb(st, "modc", [128, 48], F32)
        self.gmod = self.sb(st, "gmod", [128, 16], F32)
        self.g1bc = self.sb(st, "g1bc", [128, D], F32)
        self.g2bc = self.sb(st, "g2bc", [128, D], F32)
        self.wr = self.sb(st, "wr", [128, 8, 64], F32)
        self.wrh = self.sb(st, "wrh", [128, 8, 64], BF16)
        self.wrl = self.sb(st, "wrl", [128, 8, 64], BF16)
        self.rbias = self.sb(st, "rbias", [128, 64], F32)
        self.lam = self.sb(st, "lam", [128, 4], F32)
        self.sino = self.sb(st, "sino", [128, 32, 8], F32)
        self.coso = self.sb(st, "coso", [128, 32, 8], F32)
        self.p01 = ExitStack()
        self.sinf = self.sb(self.p01, "sinf", [128, 128, 8], F32)
        self.cosf = self.sb(self.p01, "cosf", [128, 128, 8], F32)
        sc = self.new_dsem()
        bC = Buf("consts")
        self.bC = bC
        dma("sp", sc, self.colv[:], self.colv_d, W=[bC])
        dma("sp", sc, self.rowv[:], self.rowv_d, W=[bC])
        dma("sp", sc, self.idf[:], self.idf_d, W=[bC])
        dma("sp", sc, self.cmask[:], self.cmask_d, W=[bC])
        dma("sp", sc, self.posf_i[:], self.posf_d, W=[bC])
        dma("sp", sc, self.wr[:], self.w_router.rearrange("(c p) e -> p c e", p=128), W=[bC])
        dma("sp", sc, self.poso_i[:], self.poso_d, W=[bC])
        colv, rowv = self.colv, self.rowv
        with ExitStack() as ph:
            cact = self.sb(ph, "cact", [128, 8], F32)
            wada = [self.sb(ph, "wada%d" % k, [128, 8, D], F32) for k in range(2)]
            modps = self.ps(ph, "modps", [128, 512], F32)
            bcps = self.ps(ph, "bcps", [128, 2, 512], F32)
            diag = self.sb(ph, "diag", [128, 8, 128], F32)
            wtmp = self.sb(ph, "wtmp", [128, 8, 64], F32)
            tmpf = self.sb(ph, "tmpf", [128, 128, 8], F32)
            tmpi = self.sb(ph, "tmpi", [128, 128, 8], I32)
            tmpk = self.sb(ph, "tmpk", [128, 128, 8], F32)
            tmpf2 = self.sb(ph, "tmpf2", [128, 128, 8], F32)
            posf = self.sb(ph, "posf", [128, 128], F32)
            lt = self.sb(ph, "lt", [128, 128], F32)
            bcact, bmodps, bmodc, bdiag, bbc = Buf(), Buf(), Buf(), Buf(), Buf()
            bwada = [Buf(), Buf()]
            swada = [self.new_dsem(), self.new_dsem()]
            op("dve", lambda: V.memset(self.epsc[:], EPS))
            op("dve", lambda: V.memset(self.onesf[:], 1.0))
            op("dve", lambda: V.memset(self.onesb[:], 1.0))
            op("dve", lambda: V.tensor_copy(out=self.idb[:], in_=self.idf[:]), R=[bC])
            op("act", lambda: A.activation(out=cact[:], in_=colv[:, C_C:C_C + 8], func=AF.Silu), R=[bC], W=[bcact])
            for m in range(6):
                dma("sp", swada[m % 2], wada[m % 2][:],
                    self.w_ada[:, m * D:(m + 1) * D].rearrange("(k p) n -> p k n", p=128), W=[bwada[m % 2]])

                def mm(m=m):
                    last = None
                    for c in range(8):
                        for k in range(8):
                            last = T.matmul(modps[:, m * 8 + c:m * 8 + c + 1], wada[m % 2][:, k, c * 128:(c + 1) * 128],
                                            cact[:, k:k + 1], start=(k == 0), stop=(k == 7))
                    return last
                op("pe", mm, R=[bwada[m % 2], bcact], W=[bmodps])
            op("dve", lambda: V.tensor_tensor(out=self.modc[:], in0=modps[:, 0:48], in1=colv[:, C_BADA:C_BADA + 48], op=ALU.add),
               R=[bmodps, bC], W=[bmodc])
            op("dve", lambda: V.scalar_tensor_tensor(out=self.gmod[:, 0:8], in0=self.modc[:, 8:16], scalar=1.0, in1=colv[:, C_N1G:C_N1G + 8],
                                                     op0=ALU.add, op1=ALU.mult), W=[bmodc])
            op("dve", lambda: V.scalar_tensor_tensor(out=self.gmod[:, 8:16], in0=self.modc[:, 32:40], scalar=1.0, in1=colv[:, C_N2G:C_N2G + 8],
                                                     op0=ALU.add, op1=ALU.mult), W=[bmodc])
            self.bmod = bmodc
            for which, dst in ((16, self.g1bc), (40, self.g2bc)):
                def mk(which=which):
                    last = None
                    for c in range(8):
                        last = V.tensor_scalar(out=diag[:, c, :], in0=self.idf[:], scalar1=self.modc[:, which + c:which + c + 1], scalar2=None,
                                               op0=ALU.mult)
                    return last
                op("dve", mk, SR=[bmodc], W=[bdiag])

                def mmb():
                    last = None
                    for c in range(8):
                        last = T.matmul(bcps[:, c // 4, (c % 4) * 128:(c % 4 + 1) * 128], self.onesf[:], diag[:, c, :], start=True, stop=True)
                    return last
                op("pe", mmb, R=[bdiag], W=[bbc])
                op("act", lambda dst=dst: A.activation(out=dst[:], in_=bcps[:].rearrange("p a b -> p (a b)"), func=AF.Copy), R=[bbc], W=[bmodc])
            def mk2a():
                last = None
                for c in range(8):
                    last = V.tensor_scalar(out=wtmp[:, c, :], in0=self.wr[:, c, :], scalar1=self.modc[:, 24 + c:25 + c], scalar2=None, op0=ALU.mult)
                return last

            def mk2b():
                last = None
                for c in range(8):
                    last = V.tensor_scalar(out=self.wr[:, c, :], in0=self.wr[:, c, :], scalar1=self.gmod[:, 8 + c:9 + c], scalar2=None, op0=ALU.mult)
                return last
            bwr = Buf()
            op("dve", mk2a, SR=[bmodc], R=[bC, bwr], W=[bdiag])
            op("dve", mk2b, SR=[bmodc], R=[bC], W=[bwr])
            op("dve", lambda: V.tensor_copy(out=self.wrh[:], in_=self.wr[:]), W=[bwr])
            op("dve", lambda: V.tensor_tensor(out=self.wrl[:], in0=self.wr[:], in1=self.wrh[:], op=ALU.subtract), W=[bwr])

            def mmr():
                last = None
                for c in range(8):
                    last = T.matmul(modps[:, 64:128], self.onesf[:], wtmp[:, c, :], start=(c == 0), stop=(c == 7))
                return last
            op("pe", mmr, R=[bdiag], W=[bmodps])
            op("dve", lambda: V.tensor_copy(out=self.rbias[:], in_=modps[:, 64:128]), R=[bmodps], W=[bmodc])
            lam_init = 0.8 - 0.6 * math.exp(-0.3 * 0)
            self.lam_init = lam_init

            bl = Buf()
            op("dve", lambda: V.tensor_tensor(out=lt[:, 0:64], in0=rowv[:, R_LAM:R_LAM + 64], in1=rowv[:, R_LAM + 64:R_LAM + 128], op=ALU.mult),
               R=[bC], W=[bl])
            op("dve", lambda: V.tensor_tensor(out=lt[:, 64:128], in0=rowv[:, R_LAM + 128:R_LAM + 192], in1=rowv[:, R_LAM + 192:R_LAM + 256], op=ALU.mult),
               R=[bC], W=[bl])
            op("dve", lambda: V.tensor_reduce(out=self.lam[:, 2:4], in_=lt[:].rearrange("p (a b) -> p a b", b=64), axis=AX.X, op=ALU.add), W=[bl])
            op("act", lambda: A.activation(out=self.lam[:, 2:4], in_=self.lam[:, 2:4], func=AF.Exp), W=[bl])
            op("dve", lambda: V.tensor_tensor(out=self.lam[:, 0:1], in0=self.lam[:, 2:3], in1=self.lam[:, 3:4], op=ALU.subtract), W=[bl])
            op("dve", lambda: V.tensor_scalar(out=self.lam[:, 0:1], in0=self.lam[:, 0:1], scalar1=lam_init, scalar2=None, op0=ALU.add), W=[bl])
            op("dve", lambda: V.tensor_scalar(out=self.lam[:, 1:2], in0=self.lam[:, 0:1], scalar1=-1.0, scalar2=None, op0=ALU.mult), W=[bl])
            self.blam = bl
            bt = Buf()
            for (pos_i, nb, sin_t, cos_t) in ((self.posf_i, 128, self.sinf, self.cosf), (self.poso_i, 32, self.sino, self.coso)):

                op("dve", lambda pos_i=pos_i, nb=nb: V.tensor_copy(out=posf[:, 0:nb], in_=pos_i[:]), R=[bC], W=[bt])
                op("dve", lambda nb=nb: V.tensor_tensor(out=tmpf[:, 0:nb, :], in0=posf[:, 0:nb].unsqueeze(2).to_broadcast([128, nb, 8]),
                                                        in1=colv[:, C_INVF:C_INVF + 8].unsqueeze(1).to_broadcast([128, nb, 8]), op=ALU.mult),
                   R=[bC], W=[bt])
                for shift, dst in ((0.0, sin_t), (0.5 * math.pi, cos_t)):
                    tk, tf2, ti = tmpk[:, 0:nb, :], tmpf2[:, 0:nb, :], tmpi[:, 0:nb, :]
                    tf = tmpf[:, 0:nb, :]
                    op("dve", lambda tk=tk, tf=tf, shift=shift: V.tensor_scalar(out=tk, in0=tf, scalar1=shift, scalar2=1.0 / TWO_PI, op0=ALU.add, op1=ALU.mult), W=[bt])
                    op("dve", lambda tk=tk, ti=ti: V.tensor_copy(out=ti, in_=tk), W=[bt])
                    op("dve", lambda tk=tk, ti=ti: V.tensor_copy(out=tk, in_=ti), W=[bt])
                    op("dve", lambda tk=tk, tf=tf, tf2=tf2: V.scalar_tensor_tensor(out=tf2, in0=tk, scalar=-6.28125, in1=tf, op0=ALU.mult, op1=ALU.add), W=[bt])
                    op("dve", lambda tk=tk, tf2=tf2: V.scalar_tensor_tensor(out=tk, in0=tk, scalar=-(TWO_PI - 6.28125), in1=tf2, op0=ALU.mult, op1=ALU.add), W=[bt])
                    op("dve", lambda tk=tk, shift=shift: V.tensor_scalar(out=tk, in0=tk, scalar1=shift, scalar2=math.pi, op0=ALU.add, op1=ALU.min), W=[bt])
                    op("dve", lambda tk=tk: V.tensor_scalar(out=tk, in0=tk, scalar1=-math.pi, scalar2=None, op0=ALU.max), W=[bt])
                    op("act", lambda dst=dst, tk=tk: A.activation(out=dst[:], in_=tk, func=AF.Sin), R=[bt], W=[bC])
            self.barrier()
            if "modc" in self.dbg:
                self.dump("modc", self.modc[:]); self.dump("g1bc", self.g1bc[:]); self.dump("rbias", self.rbias[:])
                self.dump("lam", self.lam[:]); self.dump("sinf", self.sinf[:]); self.dump("coso", self.coso[:])
                self.dump("wr", self.wr[:]); self.dump("gmod", self.gmod[:])
                self.barrier()

    def rms_stats(self, x_ap, rows, junk, ss, sd, rstd, bx, bst):
        nc = self.nc
        A, V = nc.scalar, nc.vector

        self.op("act", lambda: A.activation(out=junk[0:rows, :], in_=x_ap, func=AF.Square, accum_out=ss[0:rows, :]), R=[bx], W=[bst])
        self.op("act", lambda: A.activation(out=sd[0:rows, :], in_=ss[0:rows, :], func=AF.Sqrt, scale=1.0 / D, bias=self.epsc[0:rows, :]), W=[bst])
        self.op("dve", lambda: V.reciprocal(out=rstd[0:rows, :], in_=sd[0:rows, :]), W=[bst])

    def qk_process(self, src, bsrc, gcol, cos_ap, sin_ap, kb, bkb, tmp):
        nc = self.nc
        A, V = nc.scalar, nc.vector
        sq, kn, ssk, sdk, rk, r1, r2 = tmp["sq"], tmp["kn"], tmp["ssk"], tmp["sdk"], tmp["rk"], tmp["r1"], tmp["r2"]
        bt = tmp["b"]
        g3 = lambda t: t[:].rearrange("p (g e) -> p g e", e=64)

        o = self.op
        o("dve", lambda: V.tensor_tensor(out=sq[:], in0=src[:], in1=src[:], op=ALU.mult), R=[bsrc], W=[bt])
        o("dve", lambda: V.tensor_reduce(out=ssk[:], in_=g3(sq), axis=AX.X, op=ALU.add), W=[bt])
        o("act", lambda: A.activation(out=sdk[:], in_=ssk[:], func=AF.Sqrt, scale=1.0 / 64, bias=self.epsc[:]), W=[bt])
        o("dve", lambda: V.reciprocal(out=rk[:], in_=sdk[:]), W=[bt])
        o("dve", lambda: V.tensor_tensor(out=g3(kn), in0=g3(src), in1=rk[:].unsqueeze(2).to_broadcast([128, 16, 64]), op=ALU.mult), R=[bsrc], W=[bt])
        o("dve", lambda: V.tensor_tensor(out=g3(kn), in0=g3(kn), in1=self.rowv[:, gcol:gcol + 64].unsqueeze(1).to_broadcast([128, 16, 64]), op=ALU.mult), W=[bt])
        a_, b_ = g3(kn)[:, :, 0:8], g3(kn)[:, :, 8:16]
        c_ = cos_ap.unsqueeze(1).to_broadcast([128, 16, 8])
        s_ = sin_ap.unsqueeze(1).to_broadcast([128, 16, 8])
        b1, b2 = tmp["b1"], tmp["b2"]
        o("dve", lambda: V.tensor_copy(out=kb[:], in_=kn[:]), R=[bt], W=[bkb])
        o("dve", lambda: V.tensor_tensor(out=r1[:], in0=a_, in1=c_, op=ALU.mult), R=[bt], W=[b1])
        o("dve", lambda: V.tensor_tensor(out=r2[:], in0=b_, in1=s_, op=ALU.mult), R=[bt], W=[b2])
        o("dve", lambda: V.tensor_tensor(out=g3(kb)[:, :, 0:8], in0=r1[:], in1=r2[:], op=ALU.subtract), R=[b1, b2], W=[bkb])
        o("dve", lambda: V.tensor_tensor(out=r1[:], in0=b_, in1=c_, op=ALU.mult), R=[bt], W=[b1])
        o("dve", lambda: V.tensor_tensor(out=r2[:], in0=a_, in1=s_, op=ALU.mult), R=[bt], W=[b2])
        o("dve", lambda: V.tensor_tensor(out=g3(kb)[:, :, 8:16], in0=r1[:], in1=r2[:], op=ALU.add), R=[b1, b2], W=[bkb])

    def qk_tmp(self, st):
        return {"sq": self.sb(st, "sq", [128, D], F32), "kn": self.sb(st, "kn", [128, D], F32),
                "ssk": self.sb(st, "ssk", [128, 16], F32), "sdk": self.sb(st, "sdk", [128, 16], F32),
                "rk": self.sb(st, "rk", [128, 16], F32), "r1": self.sb(st, "r1", [128, 16, 8], F32),
                "r2": self.sb(st, "r2", [128, 16, 8], F32), "b": Buf("qktmp"), "b1": Buf(), "b2": Buf()}

    def phase1(self):
        nc = self.nc
        V, A, P, T = nc.vector, nc.scalar, nc.gpsimd, nc.tensor
        op, dma = self.op, self.dma
        NTL = self.cfg.get("n_p1_tiles", 32)
        with ExitStack() as ph:
            wkv = self.sb(ph, "wkv", [128, 4, 8, 512], BF16)
            xt = [self.sb(ph, "xt%d" % k, [128, 4, D], F32) for k in range(2)]
            xn = [self.sb(ph, "xn%d" % k, [128, D], BF16) for k in range(2)]
            junk = self.sb(ph, "junk", [128, D], BF16)
            stt = [[self.sb(ph, "st%d_%d" % (k, q), [128, 1], F32) for q in range(3)] for k in range(2)]
            hT = [self.sb(ph, "hT%d" % k, [128, 8, 512], BF16) for k in range(2)]
            kf = [self.sb(ph, "kf%d" % k, [128, D], F32) for k in range(2)]
            kb = [self.sb(ph, "kb%d" % k, [128, D], BF16) for k in range(2)]
            kTs = [self.sb(ph, "kTs%d" % k, [128, 8, 512], BF16) for k in range(2)]
            vst = [self.sb(ph, "vst%d" % k, [128, 8, 4, 128], BF16) for k in range(2)]
            tmp = self.qk_tmp(ph)
            tp = [self.ps(ph, "tp%d" % k, [128, 8, 128], BF16) for k in range(2)]
            kps = self.ps(ph, "kps", [128, D], F32)
            vps = self.ps(ph, "vps", [128, D], F32)
            tpk = self.ps(ph, "tpk", [128, 8, 128], BF16)
            bwkv = Buf()
            bxt, bxn, bst, bhT, bkf, bkb, bkTs, bvst, btp = ([Buf(), Buf()] for _ in range(9))
            bkps, bvps, btpk = Buf(), Buf(), Buf()
            sw = self.new_dsem()
            sx = [self.new_dsem(), self.new_dsem()]
            sk = [self.new_dsem(), self.new_dsem()]
            sv = [self.new_dsem(), self.new_dsem()]
            for pc in range(4):
                dma("sp", sw, wkv[:, pc], self.winb[:, 3072 + pc * 512:3072 + (pc + 1) * 512].rearrange("(k p) n -> p k n", p=128),
                    R=[self.bA], W=[bwkv])

            def load(g):
                dma("sp", sx[g % 2], xt[g % 2][:], self.xfull[g * 512:(g + 1) * 512, :].rearrange("(s p) d -> p s d", p=128), W=[bxt[g % 2]])
            load(0)
            NSB = NTL * 4

            def stA(n):
                g, s_ = divmod(n, 4)
                gp, q = g % 2, n % 2
                if s_ == 0 and g + 1 < NTL:
                    load(g + 1)
                ss, sd, rstd = stt[q]
                self.rms_stats(xt[gp][:, s_, :], 128, junk, ss, sd, rstd, bxt[gp], bst[q])
                op("dve", lambda: V.tensor_scalar(out=xn[q][:], in0=xt[gp][:, s_, :], scalar1=rstd[:], scalar2=None, op0=ALU.mult),
                   R=[bxt[gp]], SR=[bst[q]], W=[bxn[q]])

            def stB(n):
                g, s_ = divmod(n, 4)
                gp, q = g % 2, n % 2

                def tr():
                    last = None
                    for c in range(8):
                        last = T.transpose(out=tp[q][:, c, :], in_=xn[q][:, c * 128:(c + 1) * 128], identity=self.idb[:])
                    return last
                op("pe", tr, R=[bxn[q]], W=[btp[q]])

                def ev():
                    last = None
                    for c in range(8):
                        last = A.activation(out=hT[gp][:, c, s_ * 128:(s_ + 1) * 128], in_=tp[q][:, c, :], func=AF.Identity,
                                            scale=self.gmod[:, c:c + 1], bias=self.modc[:, c:c + 1])
                    return last
                op("act", ev, R=[btp[q]], W=[bhT[gp]])

            def stC(n):
                g, s_ = divmod(n, 4)
                gp, q = g % 2, n % 2

                def mmk():
                    last = None
                    for pc in range(2):
                        for k in range(8):
                            last = T.matmul(kps[:, pc * 512:(pc + 1) * 512], hT[gp][:, k, s_ * 128:(s_ + 1) * 128], wkv[:, pc, k, :],
                                            start=(k == 0), stop=(k == 7))
                    return last
                op("pe", mmk, R=[bhT[gp], bwkv], W=[bkps])
                op("act", lambda: A.activation(out=kf[q][:], in_=kps[:], func=AF.Copy), R=[bkps], W=[bkf[q]])

                def mmv():
                    last = None
                    for pc in range(2):
                        for k in range(8):
                            last = T.matmul(vps[:, pc * 512:(pc + 1) * 512], hT[gp][:, k, s_ * 128:(s_ + 1) * 128], wkv[:, 2 + pc, k, :],
                                            start=(k == 0), stop=(k == 7))
                    return last
                op("pe", mmv, R=[bhT[gp], bwkv], W=[bvps])
                op("act", lambda: A.activation(out=vst[gp][:, :, s_, :], in_=vps[:].rearrange("p (h v) -> p h v", v=128), func=AF.Copy),
                   R=[bvps], W=[bvst[gp]])
                self.qk_process(kf[q], bkf[q], R_KG, self.cosf[:, n, :], self.sinf[:, n, :], kb[q], bkb[q], tmp)

            def stD(n):
                g, s_ = divmod(n, 4)
                gp, q = g % 2, n % 2

                def trk():
                    last = None
                    for h in range(8):
                        last = T.transpose(out=tpk[:, h, :], in_=kb[q][:, h * 128:(h + 1) * 128], identity=self.idb[:])
                    return last
                op("pe", trk, R=[bkb[q]], W=[btpk])
                op("act", lambda: A.activation(out=kTs[gp][:, :, s_ * 128:(s_ + 1) * 128], in_=tpk[:], func=AF.Copy), R=[btpk], W=[bkTs[gp]])
                if s_ == 3:
                    dma("sp", sk[gp], self.KT.rearrange("h p t -> p h t")[:, :, g * 512:(g + 1) * 512], kTs[gp][:], R=[bkTs[gp]])
                    dma("sp", sv[gp], self.VS.rearrange("h p b v -> p h b v")[:, :, g * 4:(g + 1) * 4, :], vst[gp][:], R=[bvst[gp]])
            for n in range(NSB + 3):
                if n < NSB:
                    stA(n)
                if 0 <= n - 1 < NSB:
                    stB(n - 1)
                if 0 <= n - 2 < NSB:
                    stC(n - 2)
                if 0 <= n - 3 < NSB:
                    stD(n - 3)
            self.barrier()
            if "KT" in self.dbg:
                self.dump("KT", self.KT[:, :, 0:2048]); self.dump("VS", self.VS[:, :, 0:16, :])
                self.barrier()
        self.p01.close()

    def convert_experts(self):
        NE = self.cfg.get("n_exp", NEXP)
        for e in (list(range(NE - 1)) + [NEXP - 1] if NE < NEXP else list(range(NEXP))):
            self.dma("pool", self.sE, self.wgub[e], self.w_gu[e], W=[self.bE])
            self.dma("pool", self.sE, self.wdnb[e], self.w_dn[e], W=[self.bE])

    def dump(self, name, src):
        sid = self.new_dsem()
        self.dma("sp", sid, self.dbg[name], src)

    def finish(self):
        self.barrier()
        self.root.close()


def host_inputs(inputs, cfg):
    f = lambda k: np.asarray(inputs[k], dtype=np.float32)
    x, c = f("x"), f("c")
    pos = np.asarray(inputs["positions"]).astype(np.int32)
    col = lambda v: np.ascontiguousarray(v.reshape(-1, 128).T)
    rep = lambda v: np.ascontiguousarray(np.broadcast_to(v.reshape(1, -1), (128, v.size)))
    inv_freq = (500000.0 ** (-np.arange(0, 16, 2, dtype=np.float32) / 16)).astype(np.float32)
    w_gu = np.concatenate([f("w_exp_gu")[0], f("w_sh_gu")[0][None]], 0)
    w_dn = np.concatenate([f("w_exp_down")[0], f("w_sh_down")[0][None]], 0)
    shared = {
        "idf": np.eye(128, dtype=np.float32), "w_ada": f("w_ada")[0], "w_in": f("w_in")[0], "w_pw2": f("w_pw2")[0],
        "w_out": f("w_out")[0], "w_router": f("w_router")[0], "w_gu": w_gu, "w_dn": w_dn,
    }
    rowv = np.concatenate([rep(f("q_norm_g")[0]), rep(f("k_norm_g")[0]), rep(f("subln_g")[0]),
                           rep(f("lambda_q1")[0]), rep(f("lambda_k1")[0]), rep(f("lambda_q2")[0]), rep(f("lambda_k2")[0]),
                           rep(f("router_bias")[0])], 1)
    assert rowv.shape == (128, NROW)
    dw = f("conv_dw")[0]
    dwc = np.ascontiguousarray(dw.T.reshape(8, 128, 31).transpose(1, 0, 2).reshape(128, 248))
    maps = []
    kk = np.arange(2048)[:, None]
    qq = np.arange(512)[None, :]
    for core in range(8):
        b, j = core // 4, core % 4
        colv = np.concatenate([col(c[b]), col(f("b_ada")[0]), col(f("norm1_g")[0]), col(f("norm2_g")[0]), col(f("conv_dw_b")[0]),
                               col(f("conv_ln_g")[0]), col(f("conv_ln_b")[0]), col(f("b_pw2")[0]), dwc,
                               np.full((128, 1), 0.0 if j == 0 else 1.0, np.float32), rep(inv_freq), col(f("subln_g")[0])], 1).astype(np.float32)
        assert colv.shape == (128, NCOL)
        xown = np.zeros((8, 542, D), np.float32)
        poso = np.zeros((128, 32), np.int32)
        for i in range(8):
            t0 = 512 * (4 * i + j)
            lo = max(t0 - 30, 0)
            xown[i, 542 - (t0 + 512 - lo):] = x[b, lo:t0 + 512]
            poso[:, i * 4:(i + 1) * 4] = pos[b, t0:t0 + 512].reshape(4, 128).T
        cm = (kk <= 512 * j + qq).astype(np.float32).reshape(16, 128, 512).transpose(1, 0, 2)
        m = dict(shared)
        m.update({"xfull": x[b], "xown": xown, "colv": colv, "rowv": rowv, "posf": np.ascontiguousarray(pos[b].reshape(128, 128).T),
                  "poso": poso, "cmask": np.ascontiguousarray(cm).astype(ml_dtypes.bfloat16)})
        maps.append(m)
    return maps


def _phase23(self):
    nc = self.nc
    V, A, P, T = nc.vector, nc.scalar, nc.gpsimd, nc.tensor
    op, dma = self.op, self.dma
    cfg = self.cfg
    NT = cfg.get("n_own_tiles", 8)
    NH = cfg.get("n_heads", 8)
    colv, rowv = self.colv, self.rowv
    lam_scale = 1.0 - self.lam_init
    with ExitStack() as tl:
        QT = self.sb(tl, "QT", [128, 8, 512], BF16)
        mcT = self.sb(tl, "mcT", [128, 8, 512], BF16)
        sgaT = self.sb(tl, "sgaT", [128, 8, 512], BF16)
        yaT = self.sb(tl, "yaT", [128, 8, 512], BF16)
        bQT, bmc, bsga, bya = Buf(), Buf(), Buf(), Buf()
        sxr = self.new_dsem()
        NWB = 3
        wpc = [self.sb(tl, "wpc%d" % k, [128, 8, 512], BF16) for k in range(NWB)]
        bwp = [Buf() for _ in range(NWB)]
        swp = [self.new_dsem() for _ in range(NWB)]
        wctr = [0]

        def wload(src2d):
            k = wctr[0] % NWB
            wctr[0] += 1
            dma("sp", swp[k], wpc[k][:], src2d.rearrange("(k p) n -> p k n", p=128), R=[self.bA], W=[bwp[k]])
            return wpc[k], bwp[k]

        for i in range(NT):
            with ExitStack() as ph:
                nrm = ExitStack()
                hTo = self.sb(ph, "hTo", [128, 8, 544], BF16)
                stt = [[self.sb(ph, "st%d_%d" % (k, q), [128, 1], F32) for q in range(3)] for k in range(2)]
                tp = [self.ps(nrm, "tp%d" % k, [128, 8, 128], BF16) for k in range(2)]
                xo = self.sb(nrm, "xo", [128, 5, D], F32)
                xn = [self.sb(nrm, "xn%d" % k, [128, D], BF16) for k in range(2)]
                junk = self.sb(nrm, "junk", [128, D], BF16)
                bxo, bhTo, bu, bcv, bmean, bmsq, bvar, brstd, bzT, bpa, bpb = (Buf() for _ in range(11))
                bxn, bst, bsgb, bvsq, btn, bsgc, bqf, bqb, btp, bpg = ([Buf(), Buf()] for _ in range(10))
                sxo = self.new_dsem() if i == 0 else self._sxo
                self._sxo = sxo
                dma("sp", sxo, xo[:, 0:4, :], self.xown[i, 0:512, :].rearrange("(s p) d -> p s d", p=128), W=[bxo])
                dma("sp", sxo, xo[0:30, 4, :], self.xown[i, 512:542, :], W=[bxo])
                def nA(s):
                    rows = 128 if s < 4 else 30
                    q = s % 2
                    ss, sd, rstd = stt[q]
                    self.rms_stats(xo[0:rows, s, :], rows, junk, ss, sd, rstd, bxo, bst[q])
                    op("dve", lambda: V.tensor_scalar(out=xn[q][0:rows, :], in0=xo[0:rows, s, :], scalar1=rstd[0:rows, :], scalar2=None, op0=ALU.mult),
                       R=[bxo], SR=[bst[q]], W=[bxn[q]])

                def nB(s):
                    rows = 128 if s < 4 else 30
                    q = s % 2

                    def tr():
                        last = None
                        for c in range(8):
                            last = T.transpose(out=tp[q][:, c, 0:rows], in_=xn[q][0:rows, c * 128:(c + 1) * 128], identity=self.idb[0:rows, 0:rows])
                        return last
                    op("pe", tr, R=[bxn[q]], W=[btp[q]])

                    def ev():
                        last = None
                        for c in range(8):
                            last = A.activation(out=hTo[:, c, s * 128:s * 128 + rows], in_=tp[q][:, c, 0:rows], func=AF.Identity,
                                                scale=self.gmod[:, c:c + 1], bias=self.modc[:, c:c + 1])
                        return last
                    op("act", ev, R=[btp[q]], W=[bhTo])
                for n in range(6):
                    if n < 5:
                        nA(n)
                    if n >= 1:
                        nB(n - 1)
                self.barrier()
                nrm.close()
                if cfg.get("cut") == 1:
                    self.cut_hit = True
                    return
                u = self.sb(ph, "u", [128, 8, 544], BF16)
                sgb = [self.sb(ph, "sgb%d" % k, [128, 544], F32) for k in range(2)]
                cv = self.sb(ph, "cv", [128, 8, 512], F32)
                vsq = [self.sb(ph, "vsq%d" % k, [128, 512], F32) for k in range(2)]
                mean = self.sb(ph, "mean", [128, 512], F32)
                msq = self.sb(ph, "msq", [128, 512], F32)
                var = self.sb(ph, "var", [128, 512], F32)
                rstdv = self.sb(ph, "rstdv", [128, 512], F32)
                tn = [self.sb(ph, "tn%d" % k, [128, 512], F32) for k in range(2)]
                zT = self.sb(ph, "zT", [128, 8, 512], BF16)
                sgc = [self.sb(ph, "sgc%d" % k, [128, 512], F32) for k in range(2)]
                qf = [self.sb(ph, "qf%d" % k, [128, D], F32) for k in range(2)]
                qb = [self.sb(ph, "qb%d" % k, [128, D], BF16) for k in range(2)]
                tmp = self.qk_tmp(ph)
                pa = self.ps(ph, "pa", [128, 2, 512], F32)
                pb = self.ps(ph, "pb", [128, 2, 512], F32)
                pg = [self.ps(ph, "pg%d" % k, [128, 512], F32) for k in range(2)]
                cvs = ExitStack()
                pcv = [self.ps(cvs, "pcv%d" % k, [128, 512], F32) for k in range(2)]
                dg = [self.sb(cvs, "dg%d" % k, [128, 31, 128], BF16) for k in range(2)]
                bpcv, bdg = [Buf(), Buf()], [Buf(), Buf()]
                wa = wb_ = None
                for cc in range(8):
                    if cc % 4 == 0:
                        wa, bwa = wload(self.winb[:, (cc // 4) * 512:(cc // 4 + 1) * 512])
                        wb_, bwb = wload(self.winb[:, 1024 + (cc // 4) * 512:1024 + (cc // 4 + 1) * 512])
                    co = (cc % 4) * 128

                    def mma(w=wa, pt=pa):
                        last = None
                        for hf in range(2):
                            for k in range(8):
                                last = T.matmul(pt[:, hf, 0:271], w[:, k, co:co + 128], hTo[:, k, hf * 271:(hf + 1) * 271], start=(k == 0), stop=(k == 7))
                        return last
                    op("pe", mma, R=[bhTo, bwa], W=[bpa])
                    op("pe", lambda: mma(wb_, pb), R=[bhTo, bwb], W=[bpb])
                    q = cc % 2
                    op("act", lambda: A.activation(out=sgb[q][:, 0:542].rearrange("p (h n) -> p h n", n=271), in_=pb[:, :, 0:271], func=AF.Sigmoid),
                       R=[bpb], W=[bsgb[q]])
                    bucc = Buf()
                    op("dve", lambda: V.tensor_tensor(out=u[:, cc, 0:542].rearrange("p (h n) -> p h n", n=271), in0=pa[:, :, 0:271],
                                                      in1=sgb[q][:, 0:542].rearrange("p (h n) -> p h n", n=271), op=ALU.mult),
                       R=[bpa, bsgb[q]], W=[bucc])
                    ce, E = "dve", V
                    if i == 0:
                        op(ce, lambda: E.tensor_scalar(out=u[:, cc, 0:30], in0=u[:, cc, 0:30], scalar1=colv[:, C_HALO:C_HALO + 1], scalar2=None, op0=ALU.mult),
                           W=[bucc])
                    bcc = Buf()
                    wofs = C_DW + cc * 31

                    def mkdg():
                        last = None
                        for tap in range(31):
                            last = V.tensor_scalar(out=dg[q][:, tap, :], in0=self.idb[:], scalar1=colv[:, wofs + tap:wofs + tap + 1], scalar2=None, op0=ALU.mult)
                        return last
                    op("dve", mkdg, W=[bdg[q]])

                    def mmcv():
                        last = None
                        for tap in range(31):
                            last = T.matmul(pcv[q][:], dg[q][:, tap, :], u[:, cc, tap:tap + 512], start=(tap == 0), stop=(tap == 30))
                        return last
                    op("pe", mmcv, R=[bdg[q], bucc], W=[bpcv[q]])
                    op("act", lambda: A.activation(out=cv[:, cc, :], in_=pcv[q][:], func=AF.Identity, bias=colv[:, C_DWB + cc:C_DWB + cc + 1]),
                       R=[bpcv[q]], W=[bcc])
                    op("act", lambda: A.activation(out=vsq[q][:], in_=cv[:, cc, :], func=AF.Square), R=[bcc], W=[bvsq[q]])
                    op("pe", lambda: T.matmul(pg[0][:], self.onesf[:], cv[:, cc, :], start=(cc == 0), stop=(cc == 7)), R=[bcc], W=[bpg[0]])
                    op("pe", lambda: T.matmul(pg[1][:], self.onesf[:], vsq[q][:], start=(cc == 0), stop=(cc == 7)), R=[bvsq[q]], W=[bpg[1]])
                    bcv.w = bcc.w
                op("dve", lambda: V.tensor_scalar(out=mean[:], in0=pg[0][:], scalar1=1.0 / D, scalar2=None, op0=ALU.mult), R=[bpg[0]], W=[bmean])
                op("dve", lambda: V.tensor_tensor(out=msq[:], in0=mean[:], in1=mean[:], op=ALU.mult), R=[bmean], W=[bmsq])
                op("dve", lambda: V.scalar_tensor_tensor(out=var[:], in0=pg[1][:], scalar=1.0 / D, in1=msq[:], op0=ALU.mult, op1=ALU.subtract),
                   R=[bpg[1], bmsq], W=[bvar])
                op("act", lambda: A.activation(out=var[:], in_=var[:], func=AF.Sqrt, bias=self.epsc[:]), W=[bvar])
                op("dve", lambda: V.reciprocal(out=rstdv[:], in_=var[:]), R=[bvar], W=[brstd])
                for cc in range(8):
                    q = cc % 2
                    op("dve", lambda: V.tensor_tensor(out=tn[q][:], in0=cv[:, cc, :], in1=mean[:], op=ALU.subtract), R=[bmean], W=[btn[q]])
                    op("dve", lambda: V.tensor_tensor(out=tn[q][:], in0=tn[q][:], in1=rstdv[:], op=ALU.mult), R=[brstd], W=[btn[q]])
                    op("act", lambda: A.activation(out=zT[:, cc, :], in_=tn[q][:], func=AF.Silu, scale=colv[:, C_LNG + cc:C_LNG + cc + 1],
                                                   bias=colv[:, C_LNB + cc:C_LNB + cc + 1]), R=[btn[q]], W=[bzT])
                if cfg.get("cut") == 2:
                    self.barrier()
                    self.cut_hit = True
                    return
                self.barrier()
                cvs.close()
                tp = [self.ps(ph, "tpq%d" % k, [128, 8, 128], BF16) for k in range(2)]
                btp = [Buf(), Buf()]
                wq = [wload(self.winb[:, 2048 + pc * 512:2048 + (pc + 1) * 512]) for pc in range(2)]
                for s in range(4):
                    q = s % 2

                    def mmq():
                        last = None
                        for pc in range(2):
                            for k in range(8):
                                last = T.matmul(pa[:, pc, :], hTo[:, k, 30 + s * 128:30 + (s + 1) * 128], wq[pc][0][:, k, :], start=(k == 0), stop=(k == 7))
                        return last
                    op("pe", mmq, R=[bhTo, wq[0][1], wq[1][1]], W=[bpa])
                    op("act", lambda: A.activation(out=qf[q][:], in_=pa[:].rearrange("p a b -> p (a b)"), func=AF.Copy), R=[bpa], W=[bqf[q]])
                    self.qk_process(qf[q], bqf[q], R_QG, self.coso[:, i * 4 + s, :], self.sino[:, i * 4 + s, :], qb[q], bqb[q], tmp)

                    def trq():
                        last = None
                        for h in range(8):
                            last = T.transpose(out=tp[q][:, h, :], in_=qb[q][:, h * 128:(h + 1) * 128], identity=self.idb[:])
                        return last
                    op("pe", trq, R=[bqb[q]], W=[btp[q]])
                    op("act", lambda: A.activation(out=QT[:, :, s * 128:(s + 1) * 128], in_=tp[q][:], func=AF.Copy), R=[btp[q]], W=[bQT])
                for gsel, base in ((0, 5120), (1, 6144)):
                    for pc in range(2):
                        wg, bwg = wload(self.winb[:, base + pc * 512:base + (pc + 1) * 512])
                        for c4 in range(4):
                            dc = pc * 4 + c4
                            q = dc % 2

                            def mmg():
                                last = None
                                for k in range(8):
                                    last = T.matmul(pg[q][:], wg[:, k, c4 * 128:(c4 + 1) * 128], hTo[:, k, 30:542], start=(k == 0), stop=(k == 7))
                                return last
                            op("pe", mmg, R=[bhTo, bwg], W=[bpg[q]])
                            if gsel == 0:
                                op("act", lambda: A.activation(out=sgc[q][:], in_=pg[q][:], func=AF.Sigmoid), R=[bpg[q]], W=[bsgc[q]])
                                op("dve", lambda: V.tensor_copy(out=mcT[:, dc, :], in_=sgc[q][:]), R=[bsgc[q]], W=[bmc])
                            else:
                                op("act", lambda: A.activation(out=sgaT[:, dc, :], in_=pg[q][:], func=AF.Sigmoid), R=[bpg[q]], W=[bsga])
                for pc in range(2):
                    wp, bwpp = wload(self.wpw2b[:, pc * 512:(pc + 1) * 512])
                    for c4 in range(4):
                        dc = pc * 4 + c4
                        q = dc % 2

                        def mmp():
                            last = None
                            for k in range(8):
                                last = T.matmul(pg[q][:], wp[:, k, c4 * 128:(c4 + 1) * 128], zT[:, k, :], start=(k == 0), stop=(k == 7))
                            return last
                        op("pe", mmp, R=[bzT, bwpp], W=[bpg[q]])
                        op("dve", lambda: V.scalar_tensor_tensor(out=mcT[:, dc, :], in0=pg[q][:], scalar=colv[:, C_BPW2 + dc:C_BPW2 + dc + 1],
                                                                 in1=mcT[:, dc, :], op0=ALU.add, op1=ALU.mult), R=[bpg[q]], W=[bmc])
                if i == 0 and "u" in self.dbg:
                    self.barrier()
                    self.dump("u", u[:, :, 0:542]); self.dump("cv", cv[:]); self.dump("zT", zT[:]); self.dump("QT", QT[:])
                    self.dump("mcT", mcT[:]); self.dump("sgaT", sgaT[:]); self.dump("hTo", hTo[:, :, 0:542])
                self.barrier()
            if cfg.get("cut") == 3:
                self.cut_hit = True
                return
            with ExitStack() as ph:
                NKB = 16 * (i + 1)
                NCH = NKB // 16
                kch = [self.sb(ph, "kch%d" % k, [128, 2048], BF16) for k in range(3)]
                vch = [self.sb(ph, "vch%d" % k, [128, 16, 128], BF16) for k in range(3)]
                bkc = [Buf() for _ in range(3)]
                skc = getattr(self, "_skc", None) or [self.new_dsem() for _ in range(3)]
                self._skc = skc
                et = [self.sb(ph, "et%d" % k, [128, 2, 512], BF16) for k in range(3)]
                bet = [Buf() for k in range(3)]
                sps = [self.ps(ph, "sps%d" % k, [128, 2, 512], F32) for k in range(2)]
                bsp = [Buf() for k in range(2)]
                Esum2 = [self.sb(ph, "Esum%d" % k, [128, 2, 512], F32) for k in range(2)]
                bEs2 = [Buf(), Buf()]
                oT = [self.ps(ph, "oT%d" % m, [128, 512], F32) for m in range(2)]
                rs = [self.ps(ph, "rs%d" % m, [128, 512], F32) for m in range(2)]
                boT, brs = [Buf(), Buf()], [Buf(), Buf()]
                R1 = self.sb(ph, "R1", [128, 512], F32)
                R2 = self.sb(ph, "R2", [128, 512], F32)
                Of = self.sb(ph, "Of", [128, 512], F32)
                O2 = self.sb(ph, "O2", [128, 512], F32)
                Osq = self.sb(ph, "Osq", [128, 512], F32)
                sgs = self.sb(ph, "sgs", [128, 1], F32)
                bR1, bR2, bOf, bO2, bOsq, bsgs = (Buf() for _ in range(6))
                op("dve", lambda: V.tensor_scalar(out=sgs[:], in0=colv[:, C_SUBLN:C_SUBLN + 1], scalar1=lam_scale, scalar2=None, op0=ALU.mult), W=[bsgs])

                def kvload(h, ch):
                    slot = ch_index[(h, ch)] % 3
                    dma("sp", skc[slot], kch[slot][:], self.KT[h, :, ch * 2048:(ch + 1) * 2048], W=[bkc[slot]])
                    dma("sp", skc[slot], vch[slot][:], self.VS[h, :, ch * 16:(ch + 1) * 16, :], W=[bkc[slot]])
                chunks = [(h, ch) for h in range(NH) for ch in range(NCH)]
                ch_index = {c: k for k, c in enumerate(chunks)}
                for c in chunks[:3]:
                    kvload(*c)
                steps = [(h, ch, kb) for (h, ch) in chunks for kb in range(16)]

                def qk(n):
                    h, ch, kb = steps[n]
                    slot = ch_index[(h, ch)] % 3
                    first = (ch == 0 and kb == 0)
                    Esum, bEs = Esum2[h % 2], bEs2[h % 2]

                    def mmqk():
                        last = None
                        for m in range(2):
                            last = T.matmul(sps[n % 2][:, m, :], kch[slot][m * 64:(m + 1) * 64, kb * 128:(kb + 1) * 128],
                                            QT[m * 64:(m + 1) * 64, h, :], start=True, stop=True)
                        return last
                    op("pe", mmqk, R=[bkc[slot], bQT], W=[bsp[n % 2]])
                    op("act", lambda: A.activation(out=et[n % 3][:], in_=sps[n % 2][:], func=AF.Exp, scale=0.125), R=[bsp[n % 2]], W=[bet[n % 3]])
                    if ch == NCH - 1:
                        op("dve", lambda: V.tensor_tensor(out=et[n % 3][:], in0=et[n % 3][:],
                                                          in1=self.cmask[:, kb, :].unsqueeze(1).to_broadcast([128, 2, 512]), op=ALU.mult), W=[bet[n % 3]])
                    if first:
                        op("dve", lambda: V.tensor_copy(out=Esum[:, 0, :], in_=et[n % 3][:, 0, :]), R=[bet[n % 3]], W=[bEs])
                    else:
                        op("dve", lambda: V.tensor_tensor(out=Esum[:, 0, :], in0=Esum[:, 0, :], in1=et[n % 3][:, 0, :], op=ALU.add), R=[bet[n % 3]], W=[bEs])

                def pv(n):
                    h, ch, kb = steps[n]
                    slot = ch_index[(h, ch)] % 3
                    first, last = (ch == 0 and kb == 0), (ch == NCH - 1 and kb == 15)
                    Esum, bEs = Esum2[h % 2], bEs2[h % 2]

                    def mmpv():
                        l_ = None
                        for m in range(2):
                            l_ = T.matmul(oT[m][:], vch[slot][:, kb, :], et[n % 3][:, m, :], start=first, stop=last)
                        return l_
                    op("pe", mmpv, R=[bet[n % 3], bkc[slot]], W=[boT[0], boT[1]])
                    op("pe", lambda: T.matmul(rs[1][:], self.onesb[:], et[n % 3][:, 1, :], start=first, stop=last), R=[bet[n % 3]], W=[brs[1]])
                    if last:
                        op("pe", lambda: T.matmul(rs[0][:], self.onesf[:], Esum[:, 0, :], start=True, stop=True), R=[bEs], W=[brs[0]])
                    if kb == 15 and ch_index[(h, ch)] + 3 < len(chunks):
                        kvload(*chunks[ch_index[(h, ch)] + 3])
                    if last:
                        op("dve", lambda: V.reciprocal(out=R1[:], in_=rs[0][:]), R=[brs[0]], W=[bR1])
                        op("dve", lambda: V.reciprocal(out=R2[:], in_=rs[1][:]), R=[brs[1]], W=[bR2])
                        op("dve", lambda: V.tensor_scalar(out=R2[:], in0=R2[:], scalar1=self.lam[:, 1:2], scalar2=None, op0=ALU.mult), W=[bR2])
                        op("dve", lambda: V.tensor_tensor(out=Of[:], in0=oT[0][:], in1=R1[:], op=ALU.mult), R=[boT[0], bR1], W=[bOf])
                        op("dve", lambda: V.tensor_tensor(out=O2[:], in0=oT[1][:], in1=R2[:], op=ALU.mult), R=[boT[1], bR2], W=[bO2])
                        op("dve", lambda: V.tensor_tensor(out=Of[:], in0=Of[:], in1=O2[:], op=ALU.add), R=[bO2], W=[bOf])
                        op("act", lambda: A.activation(out=Osq[:], in_=Of[:], func=AF.Square), R=[bOf], W=[bOsq])
                        op("pe", lambda: T.matmul(rs[0][:], self.onesf[:], Osq[:], start=True, stop=True), R=[bOsq], W=[brs[0]])
                        op("act", lambda: A.activation(out=R1[:], in_=rs[0][:], func=AF.Sqrt, scale=1.0 / 128, bias=self.epsc[:]), R=[brs[0]], W=[bR1])
                        op("dve", lambda: V.reciprocal(out=R1[:], in_=R1[:]), W=[bR1])
                        op("dve", lambda: V.scalar_tensor_tensor(out=yaT[:, h, :], in0=Of[:], scalar=sgs[:, 0:1], in1=R1[:], op0=ALU.mult, op1=ALU.mult),
                           R=[bOf, bR1], SR=[bsgs], W=[bya])
                for n in range(len(steps) + 1):
                    if n < len(steps):
                        qk(n)
                    if n >= 1:
                        pv(n - 1)
                if i == 0 and "yaT" in self.dbg:
                    self.barrier()
                    self.dump("yaT", yaT[:])
                self.barrier()
            if cfg.get("cut") == 4:
                self.cut_hit = True
                return
            with ExitStack() as ph:
                mg = self.sb(ph, "mg", [128, 8, 512], BF16)
                xres = self.sb(ph, "xres", [128, 4, D], F32)
                bxres = Buf()
                dma("sp", sxr, xres[:], self.xown[i, 30:542, :].rearrange("(s p) d -> p s d", p=128), W=[bxres])
                x1 = self.sb(ph, "x1", [128, 4, D], F32)
                t1 = self.sb(ph, "t1", [128, D], F32)
                junk = self.sb(ph, "junk", [128, D], BF16)
                stt = [[self.sb(ph, "st%d_%d" % (k, q), [128, 1], F32) for q in range(3)] for k in range(2)]
                xn2 = [self.sb(ph, "xn2_%d" % k, [128, D], F32) for k in range(2)]
                xT = [self.sb(ph, "xT%d" % k, [128, 8, 128], BF16) for k in range(2)]
                xL = [self.sb(ph, "xL%d" % k, [128, 8, 128], BF16) for k in range(2)]
                h2st = self.sb(ph, "h2st", [128, 8, 512], BF16)
                gst = self.sb(ph, "gst", [64, 512], F32)
                R_ = {k: self.sb(ph, "r_" + k, [128, 64], F32) for k in ("lg", "sc", "ch", "eq", "cm", "sel", "w")}
                Rs = {k: self.sb(ph, "rs_" + k, [128, 8], F32) for k in ("m1", "m2", "gs", "t8", "gm", "u8", "sm")}
                bR = {k: Buf() for k in list(R_) + list(Rs)}
                plg = self.ps(ph, "plg", [128, 512], F32)
                po = self.ps(ph, "po", [128, 2, 512], F32)
                tph = self.ps(ph, "tph", [128, 8, 128], BF16)
                tpl = self.ps(ph, "tpl", [128, 8, 128], BF16)
                xhi = [self.sb(ph, "xhi%d" % k, [128, D], BF16) for k in range(2)]
                xlo = [self.sb(ph, "xlo%d" % k, [128, D], BF16) for k in range(2)]
                bxhi, bxlo = [Buf(), Buf()], [Buf(), Buf()]
                btph, btpl = Buf(), Buf()
                bmg, bx1, bt1, bh2, bgst, bpo, bptf, bplg = (Buf() for _ in range(8))
                bst, bxn2, bxT = ([Buf(), Buf()] for _ in range(3))
                sst = getattr(self, "_sst", None) or [self.new_dsem() for _ in range(3)]
                self._sst = sst
                for dc in range(8):
                    e_, E = "dve", V
                    bm_ = Buf()
                    op(e_, lambda: E.tensor_tensor(out=mg[:, dc, :], in0=sgaT[:, dc, :], in1=yaT[:, dc, :], op=ALU.mult), R=[bsga, bya], W=[bm_])
                    op(e_, lambda: E.tensor_tensor(out=mg[:, dc, :], in0=mg[:, dc, :], in1=mcT[:, dc, :], op=ALU.add), R=[bmc], W=[bm_])
                    bmg.r[("x", dc)] = bm_.w
                wo = [wload(self.woutb[:, pc * 512:(pc + 1) * 512]) for pc in range(2)]
                if cfg.get("xtra"):
                    for _ in range(cfg["xtra"]):
                        op("pe", lambda: T.matmul(plg[:, 0:128], self.idb[:], self.idb[:], start=True, stop=True), W=[bplg])
                for s in range(4):
                    q = s % 2

                    def mmo():
                        last = None
                        for pc in range(2):
                            for k in range(8):
                                last = T.matmul(po[:, pc, :], mg[:, k, s * 128:(s + 1) * 128], wo[pc][0][:, k, :], start=(k == 0), stop=(k == 7))
                        return last
                    self._pre("pe", [], [bmg], [])
                    op("pe", mmo, R=[wo[0][1], wo[1][1]], W=[bpo])
                    op("dve", lambda: V.tensor_tensor(out=t1[:], in0=po[:].rearrange("p a b -> p (a b)"), in1=self.g1bc[:], op=ALU.mult), R=[bpo], W=[bt1])
                    bx1s = Buf()
                    op("dve", lambda: V.tensor_tensor(out=x1[:, s, :], in0=t1[:], in1=xres[:, s, :], op=ALU.add), R=[bt1, bxres], W=[bx1s])
                    bx1.r[("x", s)] = bx1s.w
                    if cfg.get("cut") == 5:
                        continue
                    ss, sd, rstd = stt[q]
                    self.rms_stats(x1[:, s, :], 128, junk, ss, sd, rstd, bx1s, bst[q])
                    op("dve", lambda: V.tensor_scalar(out=xn2[q][:], in0=x1[:, s, :], scalar1=rstd[:], scalar2=None, op0=ALU.mult),
                       R=[bx1s], SR=[bst[q]], W=[bxn2[q]])

                    if cfg.get("cut") == 61:
                        continue
                    op("act", lambda: A.activation(out=xhi[q][:], in_=xn2[q][:], func=AF.Copy), R=[bxn2[q]], W=[bxhi[q]])
                    op("dve", lambda: V.tensor_tensor(out=xlo[q][:], in0=xn2[q][:], in1=xhi[q][:], op=ALU.subtract), R=[bxn2[q], bxhi[q]], W=[bxlo[q]])

                    def trh():
                        last = None
                        for c in range(8):
                            last = T.transpose(out=tph[:, c, :], in_=xhi[q][:, c * 128:(c + 1) * 128], identity=self.idb[:])
                        return last
                    op("pe", trh, R=[bxhi[q]], W=[btph])

                    def evh():
                        last = None
                        for c in range(8):
                            last = A.activation(out=h2st[:, c, s * 128:(s + 1) * 128], in_=tph[:, c, :], func=AF.Identity,
                                                scale=self.gmod[:, 8 + c:9 + c], bias=self.modc[:, 24 + c:25 + c])
                        return last
                    op("act", evh, R=[btph], W=[bh2])
                    op("act", lambda: A.activation(out=xT[q][:], in_=tph[:], func=AF.Copy), R=[btph], W=[bxT[q]])

                    def trl():
                        last = None
                        for c in range(8):
                            last = T.transpose(out=tpl[:, c, :], in_=xlo[q][:, c * 128:(c + 1) * 128], identity=self.idb[:])
                        return last
                    op("pe", trl, R=[bxlo[q]], W=[btpl])
                    op("act", lambda: A.activation(out=xL[q][:], in_=tpl[:], func=AF.Copy), R=[btpl], W=[bxT[q]])
                    if cfg.get("cut") == 63:
                        continue

                    def mmr():
                        last = None
                        for pi_, (xa, wa_) in enumerate(((xT[q], self.wrh), (xL[q], self.wrh), (xT[q], self.wrl))):
                            for c in range(8):
                                last = T.matmul(plg[:, 0:64], xa[:, c, :], wa_[:, c, :], start=(pi_ == 0 and c == 0), stop=(pi_ == 2 and c == 7))
                        return last
                    if cfg.get("cut") == 65:
                        op("pe", lambda: T.matmul(po[:, 1, :], self.idb[:], mg[:, 0, :], start=True, stop=True), W=[bpo])
                        continue
                    if cfg.get("cut") == 66:
                        if s == 3:
                            op("pe", mmr, R=[bxT[q]], W=[bplg])
                        continue
                    op("pe", mmr, R=[bxT[q]], W=[bplg])
                    if cfg.get("cut") == 6:
                        continue
                    r3 = lambda t: t[:].rearrange("p (g e) -> p g e", e=8)
                    lg, sc, chh, eq, cm, sel, w_ = (R_[k] for k in ("lg", "sc", "ch", "eq", "cm", "sel", "w"))
                    m1, m2, gs, t8, gm, u8, sm = (Rs[k] for k in ("m1", "m2", "gs", "t8", "gm", "u8", "sm"))
                    B_ = bR
                    op("dve", lambda: V.tensor_tensor(out=lg[:], in0=plg[:, 0:64], in1=self.rbias[:], op=ALU.add), R=[bplg], W=[B_["lg"]])
                    op("act", lambda: A.activation(out=sc[:], in_=lg[:], func=AF.Sigmoid), R=[B_["lg"]], W=[B_["sc"]])
                    op("dve", lambda: V.tensor_tensor(out=chh[:], in0=sc[:], in1=rowv[:, R_RB:R_RB + 64], op=ALU.add), R=[B_["sc"]], W=[B_["ch"]])
                    op("dve", lambda: V.tensor_reduce(out=m1[:], in_=r3(chh), axis=AX.X, op=ALU.max), R=[B_["ch"]], W=[B_["m1"]])
                    op("dve", lambda: V.tensor_tensor(out=r3(eq), in0=r3(chh), in1=m1[:].unsqueeze(2).to_broadcast([128, 8, 8]), op=ALU.is_equal),
                       R=[B_["ch"], B_["m1"]], W=[B_["eq"]])
                    op("dve", lambda: V.scalar_tensor_tensor(out=eq[:], in0=eq[:], scalar=-1e4, in1=chh[:], op0=ALU.mult, op1=ALU.add), R=[B_["ch"]], W=[B_["eq"]])
                    op("dve", lambda: V.tensor_reduce(out=m2[:], in_=r3(eq), axis=AX.X, op=ALU.max), R=[B_["eq"]], W=[B_["m2"]])
                    op("dve", lambda: V.tensor_tensor(out=gs[:], in0=m1[:], in1=m2[:], op=ALU.add), R=[B_["m1"], B_["m2"]], W=[B_["gs"]])
                    op("dve", lambda: V.max(out=t8[:], in_=gs[:]), R=[B_["gs"]], W=[B_["t8"]])
                    op("dve", lambda: V.tensor_scalar(out=gm[:], in0=gs[:], scalar1=t8[:, 3:4], scalar2=None, op0=ALU.is_ge), R=[B_["gs"]], SR=[B_["t8"]], W=[B_["gm"]])
                    op("dve", lambda: V.tensor_scalar(out=gm[:], in0=gm[:], scalar1=-1.0, scalar2=1e4, op0=ALU.add, op1=ALU.mult), W=[B_["gm"]])
                    op("dve", lambda: V.tensor_tensor(out=r3(cm), in0=r3(chh), in1=gm[:].unsqueeze(2).to_broadcast([128, 8, 8]), op=ALU.add),
                       R=[B_["ch"], B_["gm"]], W=[B_["cm"]])
                    op("dve", lambda: V.max(out=u8[:], in_=cm[:]), R=[B_["cm"]], W=[B_["u8"]])
                    op("dve", lambda: V.tensor_scalar(out=sel[:], in0=cm[:], scalar1=u8[:, 7:8], scalar2=None, op0=ALU.is_ge), R=[B_["cm"]], SR=[B_["u8"]], W=[B_["sel"]])
                    op("dve", lambda: V.tensor_tensor(out=w_[:], in0=sc[:], in1=sel[:], op=ALU.mult), R=[B_["sc"], B_["sel"]], W=[B_["w"]])
                    op("dve", lambda: V.tensor_reduce(out=sm[:, 0:1], in_=w_[:], axis=AX.X, op=ALU.add), R=[B_["w"]], W=[B_["sm"]])
                    op("dve", lambda: V.reciprocal(out=sm[:, 1:2], in_=sm[:, 0:1]), W=[B_["sm"]])
                    op("dve", lambda: V.tensor_scalar(out=w_[:], in0=w_[:], scalar1=sm[:, 1:2], scalar2=2.5, op0=ALU.mult, op1=ALU.mult), SR=[B_["sm"]], W=[B_["w"]])
                    op("pe", lambda: T.transpose(out=plg[0:64, 128:256], in_=w_[:], identity=self.idf[:]), R=[B_["w"]], W=[bplg])
                    op("act", lambda: A.activation(out=gst[:, s * 128:(s + 1) * 128], in_=plg[0:64, 128:256], func=AF.Copy), R=[bplg], W=[bgst])
                if cfg.get("cut") in (5, 6, 61, 62, 63, 65, 66):
                    self.barrier()
                    self.dump("x1", x1[:])
                    if cfg.get("cut") == 64:
                        self.dump("h2st", h2st[:])
                    self.barrier()
                    self.cut_hit = True
                    return
                self._pre("sp", [], [bx1], [])
                dma("sp", sst[0], self.x1s[i * 512:(i + 1) * 512, :].rearrange("(s p) d -> p s d", p=128), x1[:], R=[bx1])
                dma("sp", sst[1], self.h2Ts[:, :, i * 512:(i + 1) * 512], h2st[:], R=[bh2])
                dma("sp", sst[2], self.gTs[0:64, i * 512:(i + 1) * 512], gst[:], R=[bgst])
                if i == 0 and "x1" in self.dbg:
                    self.barrier()
                    self.dump("x1", x1[:]); self.dump("h2st", h2st[:]); self.dump("gst", gst[:])
                self.barrier()


Builder.phase23 = _phase23


def _phase4(self):
    nc = self.nc
    V, A, P, T = nc.vector, nc.scalar, nc.gpsimd, nc.tensor
    op, dma = self.op, self.dma
    cfg = self.cfg
    NQ = cfg.get("n_quarters", 4)
    NE = cfg.get("n_exp", NEXP)
    elist = list(range(NE - 1)) + [NEXP - 1] if NE < NEXP else list(range(NEXP))
    with ExitStack() as ph:
        h2q = self.sb(ph, "h2q", [128, 8, 1024], BF16)
        acc = self.sb(ph, "acc", [128, 8, D], F32)
        NEB = 3
        wgu = [self.sb(ph, "wgu%d" % k, [128, 8, 512], BF16) for k in range(NEB)]
        wdn = [self.sb(ph, "wdn%d" % k, [128, 2, D], BF16) for k in range(NEB)]
        bwe = [Buf() for _ in range(NEB)]
        swe = [self.new_dsem() for _ in range(NEB)]
        gb = [self.sb(ph, "gb%d" % k, [128, 1024], F32) for k in range(2)]
        bgb = [Buf(), Buf()]
        sgb = [self.new_dsem(), self.new_dsem()]
        sg = [self.sb(ph, "sg%d" % k, [128, 512], F32) for k in range(2)]
        tt = [self.sb(ph, "tt%d" % k, [128, 512], F32) for k in range(2)]
        act = [self.sb(ph, "act%d" % k, [128, 2, 512], BF16) for k in range(2)]
        xr = [self.sb(ph, "xr%d" % k, [128, D], F32) for k in range(2)]
        ot = [self.sb(ph, "ot%d" % k, [128, D], F32) for k in range(2)]
        bsg, btt, bact, bxr, bot = ([Buf(), Buf()] for _ in range(5))
        sxr = [self.new_dsem(), self.new_dsem()]
        sot = [self.new_dsem(), self.new_dsem()]
        sh2 = self.new_dsem()
        pgu = [self.ps(ph, "pgu%d" % k, [128, 2, 512], F32) for k in range(2)]
        pd = [self.ps(ph, "pd%d" % k, [128, 2, 512], F32) for k in range(2)]
        bpgu, bpd = [Buf(), Buf()], [Buf(), Buf()]
        bh2q, bacc = Buf(), [Buf() for _ in range(8)]

        def wl(n, e):
            k = n % NEB
            dma("sp", swe[k], wgu[k][:], self.wgub[e].rearrange("(k p) n -> p k n", p=128), R=[self.bE], W=[bwe[k]])
            dma("sp", swe[k], wdn[k][:], self.wdnb[e].rearrange("(c p) n -> p c n", p=128), R=[self.bE], W=[bwe[k]])

        def gl(n, e, qt):
            k = n % 2
            if e < 64:
                dma("sp", sgb[k], gb[k][:], self.gTs[e:e + 1, qt * 1024:(qt + 1) * 1024].partition_broadcast(128), W=[bgb[k]])
        NT = cfg.get("n_own_tiles", 8)
        if NT < 2 * NQ:
            bz = Buf()
            op("dve", lambda: V.memset(acc[:], 0.0), W=[bz])
            op("dve", lambda: V.memset(h2q[:], 0.0), W=[bz])
            sz = self.new_dsem()
            for i in range(NT, 2 * NQ):
                dma("sp", sz, self.h2Ts[:, :, i * 512:(i + 1) * 512], h2q[:, :, 0:512], R=[bz])
                dma("sp", sz, self.gTs[0:64, i * 512:(i + 1) * 512], acc[0:64, 0, 0:512], R=[bz])
                dma("sp", sz, self.x1s[i * 512:(i + 1) * 512, :].rearrange("(s p) d -> p s d", p=128), acc[:, 0:4, :], R=[bz])
            self.barrier()
        n = 0
        for qt in range(NQ):
            dma("sp", sh2, h2q[:], self.h2Ts[:, :, qt * 1024:(qt + 1) * 1024], W=[bh2q])
            n0 = n
            wl(n, elist[0])
            gl(n, elist[0], qt)
            if len(elist) > 1:
                wl(n + 1, elist[1])
            pend = None
            for ei, e in enumerate(elist):
                if ei + 1 < len(elist):
                    gl(n + 1, elist[ei + 1], qt)
                wk, gk = n % NEB, n % 2
                for t in range(2):
                    ak = (2 * n + t) % 2
                    for c in range(2):
                        pk = (2 * (2 * n + t) + c) % 2

                        def mmgu():
                            last = None
                            for half in range(2):
                                for k in range(8):
                                    last = T.matmul(pgu[pk][:, half, :], wgu[wk][:, k, half * 256 + c * 128:half * 256 + (c + 1) * 128],
                                                    h2q[:, k, t * 512:(t + 1) * 512], start=(k == 0), stop=(k == 7))
                            return last
                        op("pe", mmgu, R=[bwe[wk], bh2q], W=[bpgu[pk]])
                        op("act", lambda: A.activation(out=sg[pk][:], in_=pgu[pk][:, 0, :], func=AF.Silu), R=[bpgu[pk]], W=[bsg[pk]])
                        op("dve", lambda: V.tensor_tensor(out=tt[pk][:], in0=pgu[pk][:, 1, :], in1=sg[pk][:], op=ALU.mult), R=[bpgu[pk], bsg[pk]], W=[btt[pk]])
                        if e < 64:
                            op("pool", lambda: P.tensor_tensor(out=act[ak][:, c, :], in0=tt[pk][:], in1=gb[gk][:, t * 512:(t + 1) * 512], op=ALU.mult),
                               R=[btt[pk], bgb[gk]], W=[bact[ak]])
                        else:
                            op("pool", lambda: P.tensor_copy(out=act[ak][:, c, :], in_=tt[pk][:]), R=[btt[pk]], W=[bact[ak]])
                    cur = (ak, wk, t, ei == 0)
                    if pend is not None:
                        self._down(pend, act, wdn, pd, bpd, bact, bwe, acc, bacc, T, V)
                    pend = cur
                    if t == 0 and ei + 2 < len(elist):
                        wl(n + 2, elist[ei + 2])
                n += 1
            self._down(pend, act, wdn, pd, bpd, bact, bwe, acc, bacc, T, V)
            for sb_ in range(8):
                k = sb_ % 2
                r0 = qt * 1024 + sb_ * 128
                dma("sp", sxr[k], xr[k][:], self.x1s[r0:r0 + 128, :], W=[bxr[k]])
                op("dve", lambda: V.tensor_tensor(out=ot[k][:], in0=acc[:, sb_, :], in1=self.g2bc[:], op=ALU.mult), R=[bacc[sb_]], W=[bot[k]])
                op("dve", lambda: V.tensor_tensor(out=ot[k][:], in0=ot[k][:], in1=xr[k][:], op=ALU.add), R=[bxr[k]], W=[bot[k]])
                dma("sp", sot[k], self.y[r0:r0 + 128, :], ot[k][:], R=[bot[k]])
            self.barrier()


def _down(self, item, act, wdn, pd, bpd, bact, bwe, acc, bacc, T, V):
    ak, wk, t, first = item
    for s in range(4):
        dk = s % 2

        def mmd():
            last = None
            for pc in range(2):
                for c in range(2):
                    last = T.matmul(pd[dk][:, pc, :], act[ak][:, c, s * 128:(s + 1) * 128], wdn[wk][:, c, pc * 512:(pc + 1) * 512],
                                    start=(c == 0), stop=(c == 1))
            return last
        self.op("pe", mmd, R=[bact[ak], bwe[wk]], W=[bpd[dk]])
        sb_ = t * 4 + s
        src = pd[dk][:].rearrange("p a b -> p (a b)")
        if first:
            self.op("dve", lambda: V.tensor_copy(out=acc[:, sb_, :], in_=src), R=[bpd[dk]], W=[bacc[sb_]])
        else:
            self.op("dve", lambda: V.tensor_tensor(out=acc[:, sb_, :], in0=acc[:, sb_, :], in1=src, op=ALU.add), R=[bpd[dk]], W=[bacc[sb_]])


Builder.phase4 = _phase4
Builder._down = _down


def build(cfg):
    b = Builder(cfg)
    b.declare()
    b.phase0()
    stop = cfg.get("stop", 99)
    if stop >= 1:
        b.phase1()
    b.convert_experts()
    if stop >= 2:
        b.phase23()
    if stop >= 3 and not getattr(b, "cut_hit", False):
        b.phase4()
    b.finish()
    return b


def kernel(**inputs):
    cfg = {}
    b = build(cfg)
    maps = host_inputs(inputs, cfg)
    res = run_bass_kernel_spmd(b.nc, maps, core_ids=list(range(8)))
    out = np.zeros((2, S, D), np.float32)
    for core in range(8):
        bb, j = core // 4, core % 4
        y = np.asarray(res.results[core]["y"], dtype=np.float32)
        for i in range(8):
            t0 = 512 * (4 * i + j)
            out[bb, t0:t0 + 512] = y[i * 512:(i + 1) * 512]
    return out
```
